# Optimizing a Trainium2 kernel written in Bass

```python
import math
import jax, jax.numpy as jnp
from jax import lax
import numpy as np

D_MODEL = 2048
BATCH = 2
SEQ = 16384
DEPTH = 1
DEC_BATCH = 8
DEC_SEQ = 64
PAST_LEN = 1024

CHUNK = 64
Q_BLOCK = 128
ATTN_WIDTH = D_MODEL // 2
CONV_WIDTH = D_MODEL - ATTN_WIDTH
N_HEADS = 8
HEAD_DIM = ATTN_WIDTH // (2 * N_HEADS)
V_DIM = 2 * HEAD_DIM
CONV_K = 3
CONV_GROUPS = 16
N_EXPERTS = 32
TOP_K = 4
D_FF = D_MODEL
SWIGLU_ALPHA = 1.702
SWIGLU_LIMIT = 7.0
NORM_EPS = 1e-6
IN_COLS = 3 * ATTN_WIDTH + 3 * CONV_WIDTH

kernel_name = "diffattn_shortconv_moe_streaming_encoder"


def rms_norm(x, g):
    xf = x.astype(jnp.float32)
    y = xf * lax.rsqrt(jnp.mean(xf * xf, axis=-1, keepdims=True) + NORM_EPS)
    return (y * g.astype(jnp.float32)).astype(x.dtype)


def alibi_slopes():
    return jnp.asarray(2.0 ** (-8.0 * np.arange(1, N_HEADS + 1) / N_HEADS), jnp.float32)


def diff_attend(q, k, v, pos_q, pos_k, visible, lam):
    s = jnp.einsum('bqhjd,bkhjd->bhjqk', q, k).astype(jnp.float32) * (HEAD_DIM ** -0.5)
    dist = jnp.abs(pos_q[:, None] - pos_k[None, :]).astype(jnp.float32)
    s = s - (alibi_slopes()[:, None, None] * dist)[None, :, None]
    if visible is not None:
        s = jnp.where(visible, s, -jnp.inf)
    p = jax.nn.softmax(s, axis=-1)
    a = p[:, :, 0] - lam * p[:, :, 1]
    return jnp.einsum('bhqk,bkhe->bqhe', a.astype(v.dtype), v)


def attn_prompt(q, k, v, lam):
    b, s = q.shape[0], q.shape[1]
    n_blk = s // Q_BLOCK
    qb = jnp.moveaxis(q.reshape(b, n_blk, Q_BLOCK, N_HEADS, 2, HEAD_DIM), 1, 0)
    pos_k = jnp.arange(s)

    def one(args):
        i, q_blk = args
        pos_q = i * Q_BLOCK + jnp.arange(Q_BLOCK)
        visible = (pos_k // CHUNK)[None, :] <= (pos_q // CHUNK)[:, None]
        return diff_attend(q_blk, k, v, pos_q, pos_k, visible, lam)

    o = lax.map(one, (jnp.arange(n_blk), qb))
    return jnp.moveaxis(o, 0, 1).reshape(b, s, N_HEADS, V_DIM)


def attn_sample(q, k_new, v_new, past_k, past_v, lam):
    t, p = q.shape[1], past_k.shape[1]
    k = jnp.concatenate([past_k, k_new], axis=1)
    v = jnp.concatenate([past_v, v_new], axis=1)
    pos_q = p + jnp.arange(t)
    pos_k = jnp.arange(p + t)
    return diff_attend(q, k, v, pos_q, pos_k, None, lam)


def short_conv(u_in, gate_b, gate_c, conv_w, conv_state):
    u = gate_c * u_in
    full = jnp.concatenate([conv_state, u], axis=1)
    t = u.shape[1]
    y = sum(conv_w[j] * full[:, j:j + t] for j in range(CONV_K))
    return gate_b * y, full[:, -(CONV_K - 1):]


def moe(h, w_router, b_router, w_gu, b_gu, w_down, b_down):
    t, d = h.shape
    logits = h.astype(jnp.float32) @ w_router.astype(jnp.float32) + b_router.astype(jnp.float32)
    top_v, top_e = lax.top_k(logits, TOP_K)
    gate = jax.nn.softmax(top_v, axis=-1)
    m = t * TOP_K
    rows = max(64, min(512, m // (4 * N_EXPERTS)))
    n_blocks = -(-m // rows) + N_EXPERTS
    flat_e = top_e.reshape(-1)
    counts = jnp.bincount(flat_e, length=N_EXPERTS)
    padded = ((counts + rows - 1) // rows) * rows
    cum_end = jnp.cumsum(padded)
    pstart = cum_end - padded
    start = jnp.cumsum(counts) - counts
    order = jnp.argsort(flat_e)
    sorted_e = flat_e[order]
    dest = pstart[sorted_e] + (jnp.arange(m) - start[sorted_e])
    buf_tok = jnp.full((n_blocks * rows,), t, jnp.int32).at[dest].set((order // TOP_K).astype(jnp.int32))
    buf_gate = jnp.zeros((n_blocks * rows,), jnp.float32).at[dest].set(gate.reshape(-1)[order])
    block_e = jnp.minimum(jnp.searchsorted(cum_end, jnp.arange(n_blocks) * rows, side='right'), N_EXPERTS - 1)
    h_pad = jnp.concatenate([h, jnp.zeros((1, d), h.dtype)], axis=0)
    xb = h_pad[buf_tok].reshape(n_blocks, rows, d)

    def expert_block(args):
        x_blk, e = args
        gu = x_blk @ w_gu[e] + b_gu[e]
        g, up = gu[:, :D_FF], gu[:, D_FF:]
        g = jnp.minimum(g, SWIGLU_LIMIT)
        up = jnp.clip(up, -SWIGLU_LIMIT, SWIGLU_LIMIT)
        act = (up + 1) * g * jax.nn.sigmoid(SWIGLU_ALPHA * g)
        return act @ w_down[e] + b_down[e]

    yb = lax.map(expert_block, (xb, block_e)).reshape(n_blocks * rows, d)
    y = jnp.zeros((t + 1, d), h.dtype).at[buf_tok].add(yb * buf_gate[:, None].astype(yb.dtype))
    return y[:t]


def trunk_layer(x, c, past_k, past_v, conv_state, layer_idx, g_mix, g_ffn, w_ada, b_ada, w_in,
                lambda_q1, lambda_k1, lambda_q2, lambda_k2, subln_g, conv_w, w_o,
                w_router, b_router, w_gu, b_gu, w_down, b_down):
    b, t, d = x.shape
    ada = jax.nn.silu(c) @ w_ada + b_ada
    shift1, scale1, gate1, shift2, scale2, gate2 = jnp.split(ada, 6, axis=-1)

    h = rms_norm(x, g_mix) * (1 + scale1[:, None]) + shift1[:, None]
    z = h @ w_in
    a_w, c_w = ATTN_WIDTH, CONV_WIDTH
    q, k, v, u, gb, gc = jnp.split(z, [a_w, 2 * a_w, 3 * a_w, 3 * a_w + c_w, 3 * a_w + 2 * c_w], axis=-1)
    q = q.reshape(b, t, N_HEADS, 2, HEAD_DIM)
    k = k.reshape(b, t, N_HEADS, 2, HEAD_DIM)
    v = v.reshape(b, t, N_HEADS, V_DIM)

    lam_init = 0.8 - 0.6 * math.exp(-0.3 * layer_idx)
    lam = (jnp.exp(jnp.sum(lambda_q1.astype(jnp.float32) * lambda_k1.astype(jnp.float32)))
           - jnp.exp(jnp.sum(lambda_q2.astype(jnp.float32) * lambda_k2.astype(jnp.float32))) + lam_init)
    if past_k is None:
        o = attn_prompt(q, k, v, lam)
    else:
        o = attn_sample(q, k, v, past_k, past_v, lam)
    attn_out = (rms_norm(o, subln_g) * (1 - lam_init)).reshape(b, t, ATTN_WIDTH)

    conv_out, new_conv = short_conv(u, gb, gc, conv_w, conv_state)
    mix = jnp.concatenate([attn_out, conv_out], axis=-1) @ w_o
    x = x + gate1[:, None] * mix

    h2 = rms_norm(x, g_ffn) * (1 + scale2[:, None]) + shift2[:, None]
    ff = moe(h2.reshape(b * t, d), w_router, b_router, w_gu, b_gu, w_down, b_down).reshape(b, t, d)
    x = x + gate2[:, None] * ff
    return x, k, v, new_conv


def setup_inputs(seed: int = 0) -> dict:
    key = jax.random.key(seed)
    ks = jax.random.split(key, 32)
    f32 = jnp.float32
    nrm = lambda k, shape, s: jax.random.normal(k, shape, f32) * s
    D = D_MODEL
    return {
        "x_prompt": nrm(ks[0], (BATCH, SEQ, D), 1.0),
        "x_sample": nrm(ks[1], (DEC_BATCH, DEC_SEQ, D), 1.0),
        "cache_k": nrm(ks[2], (DEPTH, DEC_BATCH, PAST_LEN, N_HEADS, 2, HEAD_DIM), 1.0),
        "cache_v": nrm(ks[3], (DEPTH, DEC_BATCH, PAST_LEN, N_HEADS, V_DIM), 1.0),
        "state_conv": nrm(ks[4], (DEPTH, DEC_BATCH, CONV_K - 1, CONV_WIDTH), 1.0),
        "c_prompt": nrm(ks[5], (BATCH, D), 1.0),
        "c_sample": nrm(ks[6], (DEC_BATCH, D), 1.0),
        "g_mix": 1.0 + nrm(ks[7], (DEPTH, D), 0.02),
        "g_ffn": 1.0 + nrm(ks[8], (DEPTH, D), 0.02),
        "w_ada": nrm(ks[9], (DEPTH, D, 6 * D), 0.5 * D ** -0.5),
        "b_ada": nrm(ks[10], (DEPTH, 6 * D), 0.01),
        "w_in": nrm(ks[11], (DEPTH, D, IN_COLS), D ** -0.5),
        "lambda_q1": nrm(ks[12], (DEPTH, HEAD_DIM), 0.1),
        "lambda_k1": nrm(ks[13], (DEPTH, HEAD_DIM), 0.1),
        "lambda_q2": nrm(ks[14], (DEPTH, HEAD_DIM), 0.1),
        "lambda_k2": nrm(ks[15], (DEPTH, HEAD_DIM), 0.1),
        "subln_g": 1.0 + nrm(ks[16], (DEPTH, V_DIM), 0.02),
        "conv_w": nrm(ks[17], (DEPTH, CONV_K, CONV_WIDTH), CONV_K ** -0.5),
        "w_o": nrm(ks[18], (DEPTH, ATTN_WIDTH + CONV_WIDTH, D), (ATTN_WIDTH + CONV_WIDTH) ** -0.5),
        "w_router": nrm(ks[19], (DEPTH, D, N_EXPERTS), D ** -0.5),
        "b_router": nrm(ks[20], (DEPTH, N_EXPERTS), 0.01),
        "w_gu": nrm(ks[21], (DEPTH, N_EXPERTS, D, 2 * D_FF), D ** -0.5),
        "b_gu": nrm(ks[22], (DEPTH, N_EXPERTS, 2 * D_FF), 0.01),
        "w_down": nrm(ks[23], (DEPTH, N_EXPERTS, D_FF, D), D_FF ** -0.5),
        "b_down": nrm(ks[24], (DEPTH, N_EXPERTS, D), 0.01),
        "g_final": 1.0 + nrm(ks[25], (D,), 0.02),
    }


def reference(x_prompt, x_sample, cache_k, cache_v, state_conv, c_prompt, c_sample,
              g_mix, g_ffn, w_ada, b_ada, w_in, lambda_q1, lambda_k1, lambda_q2, lambda_k2,
              subln_g, conv_w, w_o, w_router, b_router, w_gu, b_gu, w_down, b_down, g_final):
    xp, xs = x_prompt, x_sample
    kp, vp, cp, ksl, vsl, csl = [], [], [], [], [], []
    for l in range(DEPTH):
        lp = (g_mix[l], g_ffn[l], w_ada[l], b_ada[l], w_in[l], lambda_q1[l], lambda_k1[l],
              lambda_q2[l], lambda_k2[l], subln_g[l], conv_w[l], w_o[l], w_router[l], b_router[l],
              w_gu[l], b_gu[l], w_down[l], b_down[l])
        zero_state = jnp.zeros((xp.shape[0], CONV_K - 1, CONV_WIDTH), xp.dtype)
        xp, k1, v1, s1 = trunk_layer(xp, c_prompt, None, None, zero_state, l, *lp)
        xs, k2, v2, s2 = trunk_layer(xs, c_sample, cache_k[l], cache_v[l], state_conv[l], l, *lp)
        kp.append(k1); vp.append(v1); cp.append(s1)
        ksl.append(k2); vsl.append(v2); csl.append(s2)
    y_prompt = rms_norm(xp, g_final)
    y_sample = rms_norm(xs, g_final)
    return (y_prompt, y_sample, jnp.stack(kp), jnp.stack(vp), jnp.stack(cp), jnp.stack(ksl), jnp.stack(vsl), jnp.stack(csl))
```

```python
import contextlib
from functools import partial

import numpy as np

import concourse.bass as bass
import concourse.mybir as mybir
from concourse.bass_utils import run_bass_kernel_spmd

F32 = mybir.dt.float32
BF16 = mybir.dt.bfloat16
AF = mybir.ActivationFunctionType
ALU = mybir.AluOpType

D = 2048
KC = 16
NH = 8
AW = 1024
CW = 1024
INC = 6144
DFF = 2048
EPS = 1e-6
LAM_INIT = 0.2
NEG = -30000.0


class Sched:
    def __init__(self, nc, same_eng_sync=True):
        self.nc = nc
        self.ops = []
        self.same_eng_sync = same_eng_sync
        self.mute = False
        self.eng = {"pe": nc.tensor, "act": nc.scalar, "dve": nc.vector,
                    "pool": nc.gpsimd, "sp": nc.sync}

    def op(self, eng, reads, writes, fn, *a, **kw):
        if self.mute:
            return
        self.ops.append(dict(eng=eng, fn=partial(fn, *a, **kw), reads=tuple(reads),
                             writes=tuple(writes), dma=False, lane=None, bar=False, nobar=False))

    def dma(self, q, lane, reads, writes, out, in_, nobar=False, **kw):
        if self.mute:
            return
        fn = partial(self.eng[q].dma_start, out=out, in_=in_, **kw)
        self.ops.append(dict(eng=q, fn=fn, reads=tuple(reads), writes=tuple(writes),
                             dma=True, lane=lane, bar=False, nobar=nobar))

    def barrier(self):
        if self.mute:
            return
        self.ops.append(dict(bar=True))

    def emit(self, final_wait_eng="sp"):
        nc = self.nc
        ops = self.ops
        last_w, readers = {}, {}
        n = len(ops)
        deps = [()] * n
        need = [False] * n
        last_compute, last_dma, pending = {}, {}, {}
        for i, o in enumerate(ops):
            if o["bar"]:
                bd = list(last_compute.values()) + list(last_dma.values())
                for e in self.eng:
                    pending[e] = list(bd)
                continue
            d = set()
            for r in o["reads"]:
                if r in last_w:
                    d.add(last_w[r])
            for w in o["writes"]:
                if w in last_w:
                    d.add(last_w[w])
                d.update(readers.get(w, ()))
            if o["eng"] in pending:
                d.update(pending.pop(o["eng"]))
            d.discard(i)
            keep = []
            for j in d:
                oj = ops[j]
                if (not oj["dma"]) and (not o["dma"]) and oj["eng"] == o["eng"]:
                    if o["eng"] == "pe" or not self.same_eng_sync:
                        continue
                keep.append(j)
                need[j] = True
            deps[i] = keep
            for r in o["reads"]:
                readers.setdefault(r, []).append(i)
            for w in o["writes"]:
                last_w[w] = i
                readers[w] = []
            if o["dma"]:
                if not o["nobar"]:
                    last_dma[o["lane"]] = i
            else:
                last_compute[o["eng"]] = i
        eng_sem, eng_cnt, lane_sem, lane_cnt = {}, {}, {}, {}
        ev = [None] * n
        grp = {}
        for i, o in enumerate(ops):
            if o["bar"]:
                continue
            if o["dma"]:
                ln = o["lane"]
                if ln not in lane_sem:
                    lane_sem[ln] = nc.alloc_semaphore(name="L_" + str(ln))
                    lane_cnt[ln] = 0
                lane_cnt[ln] += 16
                ev[i] = (lane_sem[ln], lane_cnt[ln])
                if o["nobar"]:
                    grp.setdefault(ln, []).append(i)
            elif need[i]:
                e = o["eng"]
                if e not in eng_sem:
                    eng_sem[e] = nc.alloc_semaphore(name="E_" + e)
                    eng_cnt[e] = 0
                eng_cnt[e] += 1
                ev[i] = (eng_sem[e], eng_cnt[e])
        for ln, idxs in grp.items():
            for i in idxs:
                ev[i] = (lane_sem[ln], lane_cnt[ln])
        waited = {}
        nw = 0
        for i, o in enumerate(ops):
            if o["bar"]:
                continue
            e = o["eng"]
            engine = self.eng[e]
            want = {}
            for j in deps[i]:
                s, v = ev[j]
                k = id(s)
                if k not in want or want[k][1] < v:
                    want[k] = (s, v)
            for k, (s, v) in want.items():
                if waited.get((e, k), 0) < v:
                    engine.wait_ge(s, v)
                    waited[(e, k)] = v
                    nw += 1
            inst = o["fn"]()
            if o["dma"]:
                inst.then_inc(lane_sem[o["lane"]], 16)
            elif need[i]:
                inst.then_inc(ev[i][0], 1)
        fe = self.eng[final_wait_eng]
        for ln, s in lane_sem.items():
            fe.wait_ge(s, lane_cnt[ln])
        self.stats = dict(n_ops=n, n_waits=nw, n_lanes=len(lane_sem), n_signals=sum(need))
        return self.stats


class Cfg:
    def __init__(self, nbs=128, ne=32, past=1024, stop=9):
        self.stop = stop
        self.NBS = nbs
        self.NSLOT = nbs // 4
        self.TOWN = self.NSLOT * 128
        self.TQ = self.TOWN + 128
        self.NE = ne
        self.PAST = past
        self.PB = past // 128
        self.SK = past + 128


def build(cfg):
    NBS, NSLOT, TOWN, TQ, NE, PAST, PB, SK = (cfg.NBS, cfg.NSLOT, cfg.TOWN, cfg.TQ, cfg.NE,
                                              cfg.PAST, cfg.PB, cfg.SK)
    SEQ = NBS * 128
    nc = bass.Bass("TRN2", target_bir_lowering=False)
    S = Sched(nc)

    def din(name, shape, dt=F32):
        return nc.dram_tensor(name, list(shape), dt, kind="ExternalInput").ap()

    def dout(name, shape, dt=F32):
        return nc.dram_tensor(name, list(shape), dt, kind="ExternalOutput").ap()

    def dscr(name, shape, dt=BF16):
        return nc.dram_tensor(name, list(shape), dt, kind="Internal").ap()

    x_all = din("x_all", [SEQ, D]); x_own = din("x_own", [TOWN, D])
    x_halo = din("x_halo", [128, D]); x_smp = din("x_smp", [128, D])
    c2 = din("c2", [2, D])
    cache_k = din("cache_k", [PAST, AW]); cache_v = din("cache_v", [PAST, AW])
    state_conv = din("state_conv", [2, CW])
    g_mix = din("g_mix", [D]); g_ffn = din("g_ffn", [D]); g_final = din("g_final", [1, D])
    w_ada = din("w_ada", [D, 6 * D]); b_ada = din("b_ada", [6 * D])
    w_in = din("w_in", [D, INC])
    lam4 = din("lam4", [4, 64]); subln_g = din("subln_g", [1, 128])
    conv_w = din("conv_w", [3, CW]); w_o = din("w_o", [D, D])
    w_router = din("w_router", [D, NE]); b_router = din("b_router", [1, NE])
    w_gu = din("w_gu", [NE, D, 2 * DFF]); b_gu = din("b_gu", [NE, 2 * DFF])
    w_down = din("w_down", [NE, DFF, D]); b_down = din("b_down", [NE, D])
    ident_d = din("ident", [128, 128])
    kaug = din("kaug", [NH, 4, SEQ]); kaug_s = din("kaug_s", [NH, 4, SK])
    qaug = din("qaug", [NH, 4, TQ])
    gbias = din("gbias", [128, NH * 4 * 128]); gbias_s = din("gbias_s", [128, NH * 128])
    hmask = din("hmask", [1, 128])

    y_own = dout("y_own", [TOWN, D]); y_smp = dout("y_smp", [128, D])
    k_own = dout("k_own", [TOWN, AW]); v_own = dout("v_own", [TOWN, AW])
    k_smp = dout("k_smp", [128, AW]); v_smp = dout("v_smp", [128, AW])
    conv_p = dout("conv_p", [2, CW]); conv_s = dout("conv_s", [2, CW])

    KT = dscr("KT", [NH, 2, 64, SEQ]); VA = dscr("VA", [SEQ, NH, 128])
    KTs = dscr("KTs", [NH, 2, 64, SK]); VAs = dscr("VAs", [SK, NH, 128])
    QT = dscr("QT", [NH, 2, 64, TQ])
    AT = dscr("AT", [NH, 128, TQ]); BT = dscr("BT", [8, 128, TQ])
    X1 = dscr("X1", [TQ, D], F32)
    H2T = dscr("H2T", [KC, 128, TQ])
    WIN = dscr("WIN", [D, INC])
    WGU = [dscr("WGU%d" % e, [D, 2 * DFF]) for e in range(NE)]
    WD = [dscr("WD%d" % e, [DFF, D]) for e in range(NE)]

    es_all = contextlib.ExitStack()
    with es_all:
        ps = [es_all.enter_context(nc.psum_tensor("ps%d" % i, [128, 512], F32)) for i in range(8)]

        def psb(i):
            return ps[i][:].bitcast(BF16)

        def sb(es, name, shape, dt):
            return es.enter_context(nc.sbuf_tensor(name, list(shape), dt))

        identf = sb(es_all, "identf", [128, 128], F32)
        identb = sb(es_all, "identb", [128, 128], BF16)
        ones_b = sb(es_all, "ones_b", [128, 128], BF16)
        ones_f = sb(es_all, "ones_f", [128, 128], F32)
        epsc = sb(es_all, "epsc", [128, 1], F32)
        adaT = sb(es_all, "adaT", [128, 96, 2], F32)
        g1p = sb(es_all, "g1p", [128, 2, KC], F32)
        sh1 = sb(es_all, "sh1", [128, 2, KC], F32)
        g2p = sb(es_all, "g2p", [128, 2, KC], F32)
        sh2 = sb(es_all, "sh2", [128, 2, KC], F32)
        gmT = sb(es_all, "gmT", [128, KC], F32)
        gfT = sb(es_all, "gfT", [128, KC], F32)
        lamt = sb(es_all, "lamt", [128, 8], F32)
        Gt = sb(es_all, "Gt", [128, NSLOT + 1, NE], F32)
        cwT = sb(es_all, "cwT", [128, 8, 3], F32)
        ugh = sb(es_all, "ugh", [128, 8, 128], F32)
        ughs = sb(es_all, "ughs", [128, 8, 2], F32)

        S.dma("sp", "c_id", [], ["identf"], identf[:], ident_d)
        S.op("dve", ["identf"], ["identb"], nc.vector.tensor_copy, out=identb[:], in_=identf[:])
        S.op("dve", [], ["ones_b"], nc.vector.memset, ones_b[:], 1.0)
        S.op("dve", [], ["ones_f"], nc.vector.memset, ones_f[:], 1.0)
        S.op("dve", [], ["epsc"], nc.vector.memset, epsc[:], EPS)


        S.dma("sp", "c_gm", [], ["gmT"], gmT[:], g_mix.rearrange("(k p) -> p k", p=128), allow_slow_non_contiguous=True)
        S.dma("sp", "c_gf", [], ["gfT"], gfT[:], g_ffn.rearrange("(k p) -> p k", p=128), allow_slow_non_contiguous=True)
        for tp in range(3):
            S.dma("sp", "c_cw", [], ["cwT"], cwT[:, :, tp], conv_w[tp].rearrange("(cb p) -> p cb", p=128), allow_slow_non_contiguous=True)
        for r_ in range(2):
            S.dma("sp", "c_sc", [], ["ughs"], ughs[:, :, r_], state_conv[r_].rearrange("(cb p) -> p cb", p=128), allow_slow_non_contiguous=True)
        for q in range(3):
            S.dma("pool", "cv_win", [], [("WIN", q)], WIN[:, q * 2048:(q + 1) * 2048],
                  w_in[:, q * 2048:(q + 1) * 2048], nobar=True)

        with contextlib.ExitStack() as es:
            cT = sb(es, "cT", [128, KC, 2], F32)
            scT = sb(es, "scT", [128, KC, 2], F32)
            baT = sb(es, "baT", [128, 96], F32)
            wsl = [sb(es, "wsl%d" % i, [128, KC, 512], F32) for i in range(4)]
            lmb = sb(es, "lmb", [128, 4, 64], F32)
            ljunk = sb(es, "ljunk", [128, 64], F32)
            lsum = sb(es, "lsum", [128, 2], F32)

            for r_ in range(2):
                S.dma("sp", "c_c2", [], ["cT"], cT[:, :, r_], c2[r_].rearrange("(k p) -> p k", p=128), allow_slow_non_contiguous=True)
            S.dma("sp", "c_ba", [], ["baT"], baT[:], b_ada.rearrange("(k p) -> p k", p=128), allow_slow_non_contiguous=True)
            S.dma("sp", "c_lm", [], ["lmb"], lmb[:].rearrange("p a b -> p (a b)"),
                  lam4.rearrange("a b -> (a b)").partition_broadcast(128))
            S.op("act", ["cT"], ["scT"], nc.scalar.activation, out=scT[:], in_=cT[:], func=AF.Silu)
            for cb4 in range(24):
                w = wsl[cb4 % 4]
                wn = "wsl%d" % (cb4 % 4)
                S.dma(("sp", "act")[cb4 % 2], wn, [], [wn], w[:],
                      w_ada[:, cb4 * 512:(cb4 + 1) * 512].rearrange("(k p) c -> p k c", p=128))
                for i in range(4):
                    cb = cb4 * 4 + i
                    for kc in range(KC):
                        S.op("pe", [wn, "scT"], ["ps0"], nc.tensor.matmul, ps[0][:, cb * 2:cb * 2 + 2],
                             lhsT=w[:, kc, i * 128:(i + 1) * 128], rhs=scT[:, kc, :],
                             start=(kc == 0), stop=(kc == KC - 1))
            S.op("dve", ["ps0", "baT"], ["adaT"], nc.vector.tensor_tensor, out=adaT[:],
                 in0=ps[0][:, 0:192].rearrange("p (k r) -> p k r", r=2),
                 in1=baT[:].unsqueeze(2).broadcast_to([128, 96, 2]), op=ALU.add)
            for r in range(2):
                S.op("dve", ["adaT"], ["sh1"], nc.vector.tensor_copy, out=sh1[:, r, :], in_=adaT[:, 0:16, r])
                S.op("dve", ["adaT"], ["sh2"], nc.vector.tensor_copy, out=sh2[:, r, :], in_=adaT[:, 48:64, r])
                S.op("dve", ["adaT", "gmT"], ["g1p"], nc.vector.scalar_tensor_tensor, out=g1p[:, r, :],
                     in0=adaT[:, 16:32, r], scalar=1.0, in1=gmT[:], op0=ALU.add, op1=ALU.mult)
                S.op("dve", ["adaT", "gfT"], ["g2p"], nc.vector.scalar_tensor_tensor, out=g2p[:, r, :],
                     in0=adaT[:, 64:80, r], scalar=1.0, in1=gfT[:], op0=ALU.add, op1=ALU.mult)
            for i in range(2):
                S.op("dve", [], ["lsum"], nc.vector.memset, lsum[:, i:i + 1], 0.0)
                S.op("dve", ["lmb", "lsum"], ["ljunk", "lsum"], nc.vector.scalar_tensor_tensor, out=ljunk[:],
                     in0=lmb[:, 2 * i, :], scalar=1.0, in1=lmb[:, 2 * i + 1, :], op0=ALU.mult, op1=ALU.mult,
                     accum_out=lsum[:, i:i + 1])
            S.op("act", ["lsum"], ["lsum2"], nc.scalar.activation, out=lamt[:, 2:4], in_=lsum[:], func=AF.Exp)
            S.op("dve", ["lsum2"], ["lamt"], nc.vector.tensor_tensor, out=lamt[:, 0:1], in0=lamt[:, 2:3],
                 in1=lamt[:, 3:4], op=ALU.subtract)
            S.op("dve", ["lamt"], ["lamt"], nc.vector.tensor_scalar, out=lamt[:, 0:1], in0=lamt[:, 0:1],
                 scalar1=LAM_INIT, scalar2=None, op0=ALU.add)
            S.op("dve", ["lamt"], ["lamt"], nc.vector.tensor_scalar, out=lamt[:, 1:2], in0=lamt[:, 0:1],
                 scalar1=-1.0, scalar2=None, op0=ALU.mult)
            S.barrier()

        S.mute = cfg.stop < 5
        for e in range(NE):
            for hf in range(2):
                S.dma("pool", "cv_gu", [], [("WGU", e, hf)],
                      WGU[e][hf * 1024:(hf + 1) * 1024, :].rearrange("r (a c) -> r a c", c=2048),
                      w_gu[e, hf * 1024:(hf + 1) * 1024, :].rearrange("r (a c) -> r a c", c=2048), nobar=True)
            S.dma("pool", "cv_d", [], [("WD", e)], WD[e], w_down[e], nobar=True)

        S.mute = cfg.stop < 1
        NTa = dict(g1p=g1p, sh1=sh1, epsc=epsc, identb=identb, psb=psb)
        with contextlib.ExitStack() as es:
            wkv = sb(es, "wkv", [128, KC, 2048], BF16)
            xts = [sb(es, "xa%d" % i, [128, D], F32) for i in range(2)]
            xbs = [sb(es, "xab%d" % i, [128, D], BF16) for i in range(2)]
            ssqs = [sb(es, "ssa%d" % i, [128, 1], F32) for i in range(2)]
            rsts = [sb(es, "rsa%d" % i, [128, 1], F32) for i in range(2)]
            junk = sb(es, "junka", [128, D], BF16)
            hTs = [sb(es, "hTa%d" % i, [128, KC, 512], BF16) for i in range(2)]
            kts = [sb(es, "kta%d" % i, [128, 512], BF16) for i in range(2)]
            vts = [sb(es, "vta%d" % i, [128, 1024], BF16) for i in range(2)]
            S.dma("sp", "wkv", [("WIN", 0), ("WIN", 1)], ["wkv"], wkv[:],
                  WIN[:, 1024:3072].rearrange("(k p) c -> p k c", p=128))
            blk_i = 0
            ev_i = 0
            for t in range(NBS // 4):
                hT = hTs[t % 2]
                hn = "hTa%d" % (t % 2)
                for b in range(4):
                    i2 = blk_i % 2
                    blk_i += 1
                    _nt(S, nc, (None, "xa%d" % i2, "xab%d" % i2, "ssa%d" % i2, "rsa%d" % i2, "junka"),
                        dict(xt=xts[i2], xb=xbs[i2], ssq=ssqs[i2], rst=rsts[i2], junk=junk),
                        x_all[(t * 4 + b) * 128:(t * 4 + b + 1) * 128, :], 0, hT, b * 128, hn, **NTa)
                for h in range(NH):
                    bank = h % 2
                    bn = "ps%d" % bank
                    for kc in range(KC):
                        S.op("pe", ["wkv", hn], [bn], nc.tensor.matmul, ps[bank][:, :],
                             lhsT=wkv[:, kc, h * 128:(h + 1) * 128], rhs=hT[:, kc, :],
                             start=(kc == 0), stop=(kc == KC - 1))
                    kt = kts[ev_i % 2]
                    kn = "kta%d" % (ev_i % 2)
                    ev_i += 1
                    S.op("act", [bn], [kn], nc.scalar.copy, out=kt[:], in_=ps[bank][:, :])
                    for tt in range(2):
                        S.dma("sp", kn + "s%d" % tt, [kn], [], KT[h, tt, :, t * 512:(t + 1) * 512],
                              kt[tt * 64:(tt + 1) * 64, :])
                for b in range(4):
                    vt = vts[b % 2]
                    vn = "vta%d" % (b % 2)
                    for half in range(2):
                        bank = 2 + half
                        bn = "ps%d" % bank
                        for kc in range(KC):
                            S.op("pe", ["wkv", hn], [bn], nc.tensor.matmul, ps[bank][:, :],
                                 lhsT=hT[:, kc, b * 128:(b + 1) * 128],
                                 rhs=wkv[:, kc, 1024 + half * 512:1024 + (half + 1) * 512],
                                 start=(kc == 0), stop=(kc == KC - 1))
                        S.op("dve", [bn], [vn], nc.vector.tensor_copy, out=vt[:, half * 512:(half + 1) * 512],
                             in_=ps[bank][:, :])
                    r0 = (t * 4 + b) * 128
                    S.dma("sp", vn + "s", [vn], [], VA[r0:r0 + 128].rearrange("r h e -> r (h e)"), vt[:])
            S.barrier()

        S.mute = cfg.stop < 2
        with contextlib.ExitStack() as es:
            wsl = [sb(es, "wb%d" % i, [128, KC, 512], BF16) for i in range(3)]
            xts = [sb(es, "xb%d" % i, [128, D], F32) for i in range(2)]
            xbs = [sb(es, "xbb%d" % i, [128, D], BF16) for i in range(2)]
            ssqs = [sb(es, "ssb%d" % i, [128, 1], F32) for i in range(2)]
            rsts = [sb(es, "rsb%d" % i, [128, 1], F32) for i in range(2)]
            junk = sb(es, "junkb", [128, D], BF16)
            hTs = [sb(es, "hTb%d" % i, [128, KC, 512], BF16) for i in range(2)]
            uT = sb(es, "uT", [128, 8, 512], F32)
            gbT = sb(es, "gbT", [128, 8, 512], F32)
            ugx = [sb(es, "ugx%d" % i, [128, 4, 130], F32) for i in range(2)]
            ycv = [sb(es, "ycv%d" % i, [128, 4, 128], F32) for i in range(2)]
            bct = [sb(es, "bct%d" % i, [128, 512], BF16) for i in range(2)]
            qts = [sb(es, "qtb%d" % i, [128, 512], BF16) for i in range(2)]
            kst = [sb(es, "kst%d" % i, [128, 512], F32) for i in range(2)]
            vbs = [sb(es, "vbs%d" % i, [128, 512], BF16) for i in range(2)]
            hmb = sb(es, "hmb", [128, 128], F32)
            cst = sb(es, "cst", [128, 8, 2], F32)
            csts = sb(es, "csts", [128, 8, 2], F32)
            cvb = sb(es, "cvb", [128, 1024], BF16)
            S.dma("sp", "c_hm", [], ["hmb"], hmb[:], hmask.partition_broadcast(128))
            S.mute = cfg.stop < 1.2
            S.dma("pool", "cv_cv", [], [], VAs[0:PAST].rearrange("r h e -> r (h e)"), cache_v)
            for pb in range(PB):
                S.dma("pool", "cvb", [], ["cvb"], cvb[:], cache_k[pb * 128:(pb + 1) * 128, :])
                for h in range(NH):
                    S.op("pe", ["cvb", "identb"], ["ps5"], nc.tensor.transpose, psb(5)[:, h * 128:(h + 1) * 128],
                         cvb[:, h * 128:(h + 1) * 128], identb[:])
                kq = qts[pb % 2]
                kqn = "qtb%d" % (pb % 2)
                for hh in range(2):
                    kq2 = [qts, vbs][hh][pb % 2]
                    kqn2 = ["qtb%d", "vbs%d"][hh] % (pb % 2)
                    S.op("act", ["ps5"], [kqn2], nc.scalar.copy, out=kq2[:], in_=psb(5)[:, hh * 512:(hh + 1) * 512])
                    for tt in range(2):
                        S.dma("sp", kqn2 + "c%d" % tt, [kqn2], [],
                              KTs[hh * 4:(hh + 1) * 4, tt, :, pb * 128:(pb + 1) * 128].rearrange("h d k -> d h k"),
                              kq2[tt * 64:(tt + 1) * 64, :].rearrange("d (h k) -> d h k", h=4))

            tiles = [("halo", x_halo, 1, 0, None)]
            for ti in range(NSLOT // 4):
                tiles.append(("own", x_own, 4, 0, ti))
            tiles.append(("smp", x_smp, 1, 1, None))
            blk_i = 0
            sl_i = 0
            ev = [0]

            def nxt(lst, names, ctr=ev):
                i = ctr[0] % len(lst)
                ctr[0] += 1
                return lst[i], names % i

            for tix, (kind, src, nbk, row, ti) in enumerate(tiles):
                S.mute = cfg.stop < dict(halo=1.4, own=1.6, smp=1.8)[kind]
                NT = nbk * 128
                hT = hTs[tix % 2]
                hn = "hTb%d" % (tix % 2)
                qc0 = TOWN if kind == "smp" else (ti * 512 if kind == "own" else 0)
                for b in range(nbk):
                    i2 = blk_i % 2
                    blk_i += 1
                    r0 = (ti * 512 + b * 128) if kind == "own" else 0
                    _nt(S, nc, (None, "xb%d" % i2, "xbb%d" % i2, "ssb%d" % i2, "rsb%d" % i2, "junkb"),
                        dict(xt=xts[i2], xb=xbs[i2], ssq=ssqs[i2], rst=rsts[i2], junk=junk),
                        src[r0:r0 + 128, :], row, hT, b * 128, hn, **NTa)
                slabs = (6, 7, 10, 11) if kind == "halo" else range(12)
                if kind == "smp" and _DEBUG.get("smp_slabs") is not None:
                    slabs = _DEBUG["smp_slabs"]
                for s in slabs:
                    w = wsl[sl_i % 3]
                    wn = "wb%d" % (sl_i % 3)
                    sl_i += 1
                    S.dma("sp", wn, [("WIN", (s * 512) // 2048)], [wn], w[:],
                          WIN[:, s * 512:(s + 1) * 512].rearrange("(k p) c -> p k c", p=128))
                    fm = s in (0, 1, 6, 7, 8, 9, 10, 11) or (kind == "smp" and s in (2, 3))
                    if fm:
                        for i in range(4):
                            bank = i % 4
                            bn = "ps%d" % bank
                            for kc in range(KC):
                                S.op("pe", [wn, hn], [bn], nc.tensor.matmul, ps[bank][:, 0:NT],
                                     lhsT=w[:, kc, i * 128:(i + 1) * 128], rhs=hT[:, kc, 0:NT],
                                     start=(kc == 0), stop=(kc == KC - 1))
                            if s in (0, 1):
                                h = 4 * s + i
                                qt, qn = nxt(qts, "qtb%d")
                                S.op("act", [bn], [qn], nc.scalar.activation, out=qt[:, 0:NT], in_=ps[bank][:, 0:NT],
                                     func=AF.Copy, scale=0.125)
                                for tt in range(2):
                                    S.dma("sp", qn + "q%d" % tt, [qn], [], QT[h, tt, :, qc0:qc0 + NT],
                                          qt[tt * 64:(tt + 1) * 64, 0:NT])
                            elif s in (2, 3):
                                h = 4 * (s - 2) + i
                                qt, qn = nxt(qts, "qtb%d")
                                S.op("act", [bn], [qn], nc.scalar.copy, out=qt[:, 0:NT], in_=ps[bank][:, 0:NT])
                                for tt in range(2):
                                    S.dma("sp", qn + "q%d" % tt, [qn], [], KTs[h, tt, :, PAST:PAST + 128],
                                          qt[tt * 64:(tt + 1) * 64, 0:128])
                            elif s in (6, 7):
                                cb = 4 * (s - 6) + i
                                S.op("act", [bn], ["uT"], nc.scalar.copy, out=uT[:, cb, 0:NT], in_=ps[bank][:, 0:NT])
                            elif s in (8, 9):
                                cb = 4 * (s - 8) + i
                                S.op("act", [bn], ["gbT"], nc.scalar.copy, out=gbT[:, cb, 0:NT], in_=ps[bank][:, 0:NT])
                            else:
                                cb = 4 * (s - 10) + i
                                if kind == "halo":
                                    S.op("dve", [bn, "uT"], ["ugh"], nc.vector.tensor_tensor, out=ugh[:, cb, :],
                                         in0=ps[bank][:, 0:128], in1=uT[:, cb, 0:128], op=ALU.mult)
                                    S.op("dve", ["ugh", "hmb"], ["ugh"], nc.vector.tensor_tensor, out=ugh[:, cb, :],
                                         in0=ugh[:, cb, :], in1=hmb[:], op=ALU.mult)
                                    continue
                                ux, uxn = nxt(ugx, "ugx%d")
                                yc, ycn = nxt(ycv, "ycv%d")
                                bc, bcn = nxt(bct, "bct%d")
                                S.op("dve", [bn, "uT"], [uxn], nc.vector.tensor_tensor, out=ux[:, 0:nbk, 2:130],
                                     in0=ps[bank][:, 0:NT].rearrange("p (b t) -> p b t", t=128),
                                     in1=uT[:, cb, 0:NT].rearrange("p (b t) -> p b t", t=128), op=ALU.mult)
                                if kind == "own":
                                    S.op("dve", ["ugh"], [uxn], nc.vector.tensor_copy, out=ux[:, 0:4, 0:2],
                                         in_=ugh[:, cb, ti * 8:ti * 8 + 8].rearrange("p (b r) -> p b r", r=2))
                                else:
                                    S.op("dve", ["ughs"], [uxn], nc.vector.tensor_copy, out=ux[:, 0, 0:2],
                                         in_=ughs[:, cb, :])
                                S.op("dve", [uxn, "cwT"], [ycn], nc.vector.tensor_scalar, out=yc[:, 0:nbk, :],
                                     in0=ux[:, 0:nbk, 2:130], scalar1=cwT[:, cb, 2:3], scalar2=None, op0=ALU.mult)
                                for tap in (1, 0):
                                    S.op("dve", [uxn, "cwT", ycn], [ycn], nc.vector.scalar_tensor_tensor,
                                         out=yc[:, 0:nbk, :], in0=ux[:, 0:nbk, tap:tap + 128],
                                         scalar=cwT[:, cb, tap:tap + 1], in1=yc[:, 0:nbk, :],
                                         op0=ALU.mult, op1=ALU.add)
                                S.op("dve", [ycn, "gbT"], [bcn], nc.vector.tensor_tensor,
                                     out=bc[:, 0:NT].rearrange("p (b t) -> p b t", t=128), in0=yc[:, 0:nbk, :],
                                     in1=gbT[:, cb, 0:NT].rearrange("p (b t) -> p b t", t=128), op=ALU.mult)
                                S.dma("sp", bcn + "s", [bcn], [], BT[cb, :, qc0:qc0 + NT], bc[:, 0:NT])
                                if kind == "own" and ti == NSLOT // 4 - 1:
                                    S.op("dve", [uxn], ["cst"], nc.vector.tensor_copy, out=cst[:, cb, :],
                                         in_=ux[:, 3, 128:130])
                                if kind == "smp":
                                    S.op("dve", [uxn], ["csts"], nc.vector.tensor_copy, out=csts[:, cb, :],
                                         in_=ux[:, 0, 64:66])
                    if s in (2, 3, 4, 5):
                        for b in range(nbk):
                            bank = _DEBUG.get("tm_bank", 4) + (b % 2)
                            bn = "ps%d" % bank
                            for kc in range(KC):
                                S.op("pe", [wn, hn], [bn], nc.tensor.matmul, ps[bank][:, :],
                                     lhsT=hT[:, kc, b * 128:(b + 1) * 128], rhs=w[:, kc, :],
                                     start=(kc == 0), stop=(kc == KC - 1))
                            ks, ksn = nxt(kst, "kst%d")
                            sk_ = _DEBUG.get("tm_skip", ())
                            if "ks" not in sk_:
                                S.op("dve", [bn], [ksn], nc.vector.tensor_copy, out=ks[:], in_=ps[bank][:, :])
                            c0 = (s % 2) * 512
                            if kind == "own":
                                r0 = ti * 512 + b * 128
                                dst = (k_own if s < 4 else v_own)[r0:r0 + 128, c0:c0 + 512]
                            else:
                                dst = (k_smp if s < 4 else v_smp)[:, c0:c0 + 512]
                            if "ksdma" not in sk_:
                                S.dma("sp", ksn + "s", [ksn], [], dst, ks[:])
                            if kind == "smp" and s in (4, 5):
                                vb, vbn = nxt(vbs, "vbs%d")
                                if "vb" not in sk_:
                                    S.op("act", [ksn], [vbn], nc.scalar.copy, out=vb[:], in_=ks[:])
                                if "vbdma" not in sk_:
                                    S.dma("sp", vbn + "s", [vbn], [],
                                          VAs[PAST:PAST + 128, (s - 4) * 4:(s - 4) * 4 + 4, :].rearrange("r h e -> r (h e)"),
                                          vb[:])

            S.mute = cfg.stop < 2
            for r_ in range(2):
                S.dma("sp", "cst_o%d" % r_, ["cst"], [], conv_p[r_].rearrange("(cb p) -> p cb", p=128), cst[:, :, r_], allow_slow_non_contiguous=True)
                S.dma("sp", "csts_o%d" % r_, ["csts"], [], conv_s[r_].rearrange("(cb p) -> p cb", p=128), csts[:, :, r_], allow_slow_non_contiguous=True)
            S.barrier()

        S.mute = cfg.stop < 3
        with contextlib.ExitStack() as es:
            KA = sb(es, "KA", [68, 2, SEQ], BF16)
            VT = sb(es, "VT", [128, NBS, 130], BF16)
            QA = sb(es, "QA", [68, 2, TQ], BF16)
            KAs = sb(es, "KAs", [68, 2, SK], BF16)
            VTs = sb(es, "VTs", [128, PB + 1, 130], BF16)
            GB = sb(es, "GB", [128, NH * 4 * 128], F32)
            GBs = sb(es, "GBs", [128, NH * 128], F32)
            PTs = [sb(es, "PT%d" % i, [128, 512], BF16) for i in range(3)]
            tmS = [sb(es, "tmS%d" % i, [128, 512], F32) for i in range(2)]
            sgb = sb(es, "sgb", [128, 128], F32)
            fin = [sb(es, "fin%d" % i, [128, 8], F32) for i in range(2)]
            otm = [sb(es, "otm%d" % i, [128, 128], F32) for i in range(2)]
            ocm = [sb(es, "ocm%d" % i, [128, 128], F32) for i in range(2)]
            ojk = sb(es, "ojk", [128, 128], F32)
            abf = [sb(es, "abf%d" % i, [128, 128], BF16) for i in range(2)]
            atT = [sb(es, "atT%d" % i, [128, 128], BF16) for i in range(2)]
            S.dma("sp", "c_gb", [], ["GB"], GB[:], gbias)
            S.dma("sp", "c_gbs", [], ["GBs"], GBs[:], gbias_s)
            S.dma("sp", "c_sg", [], ["sgb"], sgb[:], subln_g.partition_broadcast(128))
            S.op("dve", ["sgb"], ["sgb"], nc.vector.tensor_scalar, out=sgb[:], in0=sgb[:], scalar1=1.0 - LAM_INIT,
                 scalar2=None, op0=ALU.mult)
            S.op("dve", [], ["VT"], nc.vector.memset, VT[:, :, 128:130], 1.0)
            S.op("dve", [], ["VTs"], nc.vector.memset, VTs[:, :, 128:130], 1.0)
            ctr = dict(pt=0, s=0, f=0)

            def attend(h, qcol, Kt, Kn, Vt_, Vn, plain_groups, corr_blocks, corr_ap, slot_id):
                fi = ctr["f"] % 2
                ctr["f"] += 1
                obank = [4 + 0, 4 + 1] if fi == 0 else [6, 7]
                for t in range(2):
                    ob = obank[t]
                    obn = "ps%d" % ob
                    groups = [(g, False) for g in plain_groups] + [(corr_blocks, True)]
                    first = True
                    for gi, (blks, is_corr) in enumerate(groups):
                        sbank = ctr["s"] % 4
                        ctr["s"] += 1
                        sbn = "ps%d" % sbank
                        nb = len(blks)
                        for i, kb in enumerate(blks):
                            S.op("pe", [Kn, "QA"], [sbn], nc.tensor.matmul, ps[sbank][:, i * 128:(i + 1) * 128],
                                 lhsT=Kt[0:68, t, kb * 128:(kb + 1) * 128], rhs=QA[0:68, t, qcol:qcol + 128],
                                 start=True, stop=True)
                        pt = PTs[ctr["pt"] % 3]
                        ptn = "PT%d" % (ctr["pt"] % 3)
                        ctr["pt"] += 1
                        if is_corr:
                            tm = tmS[gi % 2]
                            tmn = "tmS%d" % (gi % 2)
                            S.op("dve", [sbn, "GB", "GBs"], [tmn], nc.vector.tensor_tensor, out=tm[:, 0:nb * 128],
                                 in0=ps[sbank][:, 0:nb * 128], in1=corr_ap, op=ALU.add)
                            S.op("act", [tmn], [ptn], nc.scalar.activation, out=pt[:, 0:nb * 128],
                                 in_=tm[:, 0:nb * 128], func=AF.Exp)
                        else:
                            S.op("act", [sbn], [ptn], nc.scalar.activation, out=pt[:, 0:nb * 128],
                                 in_=ps[sbank][:, 0:nb * 128], func=AF.Exp)
                        for i, kb in enumerate(blks):
                            last = (gi == len(groups) - 1) and (i == nb - 1)
                            S.op("pe", [ptn, Vn], [obn], nc.tensor.matmul, ps[ob][:, 0:129],
                                 lhsT=pt[:, i * 128:(i + 1) * 128], rhs=Vt_[:, kb, 0:129],
                                 start=first, stop=last)
                            first = False
                f = fin[fi]
                fn_ = "fin%d" % fi
                o0, o1 = ps[obank[0]], ps[obank[1]]
                o0n, o1n = "ps%d" % obank[0], "ps%d" % obank[1]
                S.op("dve", [o0n], [fn_], nc.vector.reciprocal, out=f[:, 0:1], in_=o0[:, 128:129])
                S.op("dve", [o1n], [fn_], nc.vector.reciprocal, out=f[:, 1:2], in_=o1[:, 128:129])
                S.op("dve", [fn_, "lamt"], [fn_], nc.vector.tensor_tensor, out=f[:, 1:2], in0=f[:, 1:2],
                     in1=lamt[:, 1:2], op=ALU.mult)
                ot, otn = otm[fi], "otm%d" % fi
                oc, ocn = ocm[fi], "ocm%d" % fi
                S.op("dve", [o1n, fn_], [otn], nc.vector.tensor_scalar, out=ot[:], in0=o1[:, 0:128],
                     scalar1=f[:, 1:2], scalar2=None, op0=ALU.mult)
                S.op("dve", [o0n, fn_, otn], [ocn], nc.vector.scalar_tensor_tensor, out=oc[:], in0=o0[:, 0:128],
                     scalar=f[:, 0:1], in1=ot[:], op0=ALU.mult, op1=ALU.add)
                S.op("dve", [], [fn_], nc.vector.memset, f[:, 2:3], 0.0)
                S.op("dve", [ocn, fn_], ["ojk", fn_], nc.vector.scalar_tensor_tensor, out=ojk[:], in0=oc[:],
                     scalar=1.0, in1=oc[:], op0=ALU.mult, op1=ALU.mult, accum_out=f[:, 2:3])
                S.op("act", [fn_], [fn_], nc.scalar.activation, out=f[:, 3:4], in_=f[:, 2:3], func=AF.Ln,
                     bias=epsc[:, 0:1], scale=1.0 / 128)
                S.op("act", [fn_], [fn_], nc.scalar.activation, out=f[:, 4:5], in_=f[:, 3:4], func=AF.Exp,
                     scale=-0.5)
                ab, abn = abf[fi], "abf%d" % fi
                S.op("dve", [ocn, fn_, "sgb"], [abn], nc.vector.scalar_tensor_tensor, out=ab[:], in0=oc[:],
                     scalar=f[:, 4:5], in1=sgb[:], op0=ALU.mult, op1=ALU.mult)
                tb = obank[0]
                tbn = "ps%d" % tb
                S.op("pe", [abn, "identb"], [tbn], nc.tensor.transpose, psb(tb)[:, 512:640], ab[:], identb[:])
                at, atn = atT[fi], "atT%d" % fi
                S.op("act", [tbn], [atn], nc.scalar.copy, out=at[:], in_=psb(tb)[:, 512:640])
                S.dma("sp", atn + "s", [atn], [], AT[h, :, qcol:qcol + 128], at[:])

            for h in range(NH):
                S.dma("sp", "KAd", [], ["KA"], KA[0:64, :, :], KT[h].rearrange("t d k -> d t k"))
                S.dma("sp", "QAd", [], ["QA"], QA[0:64, :, :], QT[h].rearrange("t d k -> d t k"))
                S.dma("sp", "KAsd", [], ["KAs"], KAs[0:64, :, :], KTs[h].rearrange("t d k -> d t k"))
                for t in range(2):
                    S.dma("pool", "KAa%d" % t, [], ["KA"], KA[64:68, t, :], kaug[h])
                    S.dma("pool", "QAa%d" % t, [], ["QA"], QA[64:68, t, :], qaug[h])
                    S.dma("pool", "KAsa%d" % t, [], ["KAs"], KAs[64:68, t, :], kaug_s[h])
                S.dma("sp", "VTd", [], ["VT"], VT[:, :, 0:128], VA[:, h, :].rearrange("(kb p) e -> p kb e", p=128))
                S.dma("sp", "VTsd", [], ["VTs"], VTs[:, :, 0:128], VAs[:, h, :].rearrange("(kb p) e -> p kb e", p=128))
                for m in range(NSLOT):
                    plain = [[4 * g + i for i in range(4)] for g in range(m)]
                    corr = [4 * m + i for i in range(4)]
                    attend(h, m * 128, KA, "KA", VT, "VT", plain, corr, GB[:, h * 512:(h + 1) * 512], m)
                plain = [[4 * g + i for i in range(4)] for g in range(PB // 4)]
                attend(h, TOWN, KAs, "KAs", VTs, "VTs", plain, [PB], GBs[:, h * 128:(h + 1) * 128], NSLOT)
            S.barrier()

        S.mute = cfg.stop < 4
        with contextlib.ExitStack() as es:
            WO = sb(es, "WO", [128, KC, D], BF16)
            WR = sb(es, "WR", [128, KC, NE], F32)
            brb = sb(es, "brb", [128, NE], F32)
            g1b = sb(es, "g1b", [128, 2, D], F32)
            dg = sb(es, "dg", [128, 128], F32)
            catT = [sb(es, "catT%d" % i, [128, KC, 128], BF16) for i in range(2)]
            xts = [sb(es, "xd%d" % i, [128, D], F32) for i in range(2)]
            x1s = [sb(es, "x1d%d" % i, [128, D], F32) for i in range(2)]
            xn2 = sb(es, "xn2", [128, D], F32)
            junk = sb(es, "junkd", [128, D], BF16)
            h32 = sb(es, "h32", [128, KC, 128], F32)
            h2b = [sb(es, "h2b%d" % i, [128, KC, 128], BF16) for i in range(2)]
            ssq = sb(es, "ssd", [128, 1], F32)
            rst = sb(es, "rsd", [128, 1], F32)
            lg = sb(es, "lg", [128, NE], F32)
            t8 = sb(es, "t8", [128, 8], F32)
            msk = sb(es, "msk", [128, NE], F32)
            exl = sb(es, "exl", [128, NE], F32)
            den = sb(es, "den", [128, 2], F32)
            S.dma("pool", "WOd", [], ["WO"], WO[:], w_o.rearrange("(k p) c -> p k c", p=128))
            S.dma("sp", "WRd", [], ["WR"], WR[:], w_router.rearrange("(k p) c -> p k c", p=128))
            S.dma("sp", "brd", [], ["brb"], brb[:], b_router.partition_broadcast(128))
            for r in range(2):
                for kc in range(KC):
                    S.op("dve", ["adaT", "identf"], ["dg"], nc.vector.tensor_scalar, out=dg[:], in0=identf[:],
                         scalar1=adaT[:, 32 + kc, r:r + 1], scalar2=None, op0=ALU.mult)
                    S.op("pe", ["dg", "ones_f"], ["ps0"], nc.tensor.matmul, ps[0][:, 0:128], lhsT=ones_f[:],
                         rhs=dg[:], start=True, stop=True)
                    S.op("act", ["ps0"], ["g1b"], nc.scalar.copy, out=g1b[:, r, kc * 128:(kc + 1) * 128],
                         in_=ps[0][:, 0:128])
            for blk in range(NSLOT + 1):
                row = 1 if blk == NSLOT else 0
                q0 = blk * 128
                ct, ctn = catT[blk % 2], "catT%d" % (blk % 2)
                xt, xtn = xts[blk % 2], "xd%d" % (blk % 2)
                x1, x1n = x1s[blk % 2], "x1d%d" % (blk % 2)
                hb, hbn = h2b[blk % 2], "h2b%d" % (blk % 2)
                S.dma("sp", ctn + "a", [], [ctn], ct[:, 0:8, :], AT[:, :, q0:q0 + 128].rearrange("h e q -> e h q"))
                S.dma("sp", ctn + "b", [], [ctn], ct[:, 8:16, :], BT[:, :, q0:q0 + 128].rearrange("h e q -> e h q"))
                S.dma("sp", xtn, [], [xtn], xt[:], (x_smp if row else x_own[q0:q0 + 128, :]))
                for nb_ in range(4):
                    bank = nb_ % 4
                    bn = "ps%d" % bank
                    for kc in range(KC):
                        S.op("pe", [ctn, "WO"], [bn], nc.tensor.matmul, ps[bank][:, :], lhsT=ct[:, kc, :],
                             rhs=WO[:, kc, nb_ * 512:(nb_ + 1) * 512], start=(kc == 0), stop=(kc == KC - 1))
                    S.op("dve", [bn, "g1b"], [x1n], nc.vector.tensor_tensor, out=x1[:, nb_ * 512:(nb_ + 1) * 512],
                         in0=ps[bank][:, :], in1=g1b[:, row, nb_ * 512:(nb_ + 1) * 512], op=ALU.mult)
                S.op("dve", [x1n, xtn], [x1n], nc.vector.tensor_tensor, out=x1[:], in0=x1[:], in1=xt[:], op=ALU.add)
                S.dma("sp", x1n + "s", [x1n], [], X1[q0:q0 + 128, :], x1[:])
                S.op("dve", [], ["ssd"], nc.vector.memset, ssq[:], 0.0)
                S.op("act", [x1n, "ssd"], ["junkd", "ssd"], nc.scalar.activation, out=junk[:], in_=x1[:],
                     func=AF.Square, accum_out=ssq[:, 0:1])
                S.op("act", ["ssd"], ["rsd"], nc.scalar.activation, out=rst[:], in_=ssq[:], func=AF.Sqrt,
                     bias=epsc[:, 0:1], scale=1.0 / D)
                S.op("dve", ["rsd"], ["rsd"], nc.vector.reciprocal, out=rst[:], in_=rst[:])
                S.op("dve", [x1n, "rsd"], ["xn2"], nc.vector.tensor_scalar, out=xn2[:], in0=x1[:],
                     scalar1=rst[:, 0:1], scalar2=None, op0=ALU.mult)
                for q4 in range(4):
                    bank = 4 + (q4 % 2)
                    bn = "ps%d" % bank
                    for k in range(4):
                        kc = q4 * 4 + k
                        S.op("pe", ["xn2", "identf"], [bn], nc.tensor.transpose, ps[bank][:, k * 128:(k + 1) * 128],
                             xn2[:, kc * 128:(kc + 1) * 128], identf[:])
                    dst = h32[:, q4 * 4:(q4 + 1) * 4, :]
                    S.op("dve", [bn, "g2p"], ["h32"], nc.vector.tensor_tensor, out=dst,
                         in0=ps[bank][:, :].rearrange("p (k t) -> p k t", k=4),
                         in1=g2p[:, row, q4 * 4:(q4 + 1) * 4].unsqueeze(2).broadcast_to([128, 4, 128]), op=ALU.mult)
                    S.op("dve", ["h32", "sh2"], ["h32"], nc.vector.tensor_tensor, out=dst, in0=dst,
                         in1=sh2[:, row, q4 * 4:(q4 + 1) * 4].unsqueeze(2).broadcast_to([128, 4, 128]), op=ALU.add)
                S.op("act", ["h32"], [hbn], nc.scalar.copy, out=hb[:], in_=h32[:])
                S.dma("sp", hbn + "s", [hbn], [], H2T[:, :, q0:q0 + 128].rearrange("k p t -> p k t"), hb[:])
                for kc in range(KC):
                    S.op("pe", ["h32", "WR"], ["ps6"], nc.tensor.matmul, ps[6][:, 0:NE], lhsT=h32[:, kc, :],
                         rhs=WR[:, kc, :], start=(kc == 0), stop=(kc == KC - 1))
                S.op("dve", ["ps6", "brb"], ["lg"], nc.vector.tensor_tensor, out=lg[:], in0=ps[6][:, 0:NE],
                     in1=brb[:], op=ALU.add)
                S.op("dve", ["lg"], ["t8"], nc.vector.max, out=t8[:], in_=lg[:])
                S.op("dve", ["lg", "t8"], ["msk"], nc.vector.tensor_scalar, out=msk[:], in0=lg[:],
                     scalar1=t8[:, 3:4], scalar2=None, op0=ALU.is_ge)
                S.op("dve", ["t8"], ["den"], nc.vector.tensor_scalar, out=den[:, 1:2], in0=t8[:, 0:1],
                     scalar1=-1.0, scalar2=None, op0=ALU.mult)
                S.op("act", ["lg", "den"], ["exl"], nc.scalar.activation, out=exl[:], in_=lg[:], func=AF.Exp,
                     bias=den[:, 1:2], scale=1.0)
                S.op("dve", [], ["den0"], nc.vector.memset, den[:, 0:1], 0.0)
                S.op("dve", ["exl", "msk", "den0"], ["exl", "den0"], nc.vector.scalar_tensor_tensor, out=exl[:],
                     in0=exl[:], scalar=1.0, in1=msk[:], op0=ALU.mult, op1=ALU.mult, accum_out=den[:, 0:1])
                S.op("dve", ["den0"], ["den0"], nc.vector.reciprocal, out=den[:, 0:1], in_=den[:, 0:1])
                S.op("dve", ["exl", "den0"], ["Gt"], nc.vector.tensor_scalar, out=Gt[:, blk, :], in0=exl[:],
                     scalar1=den[:, 0:1], scalar2=None, op0=ALU.mult)
            S.barrier()

        S.mute = cfg.stop < 5
        with contextlib.ExitStack() as es:
            h2T = sb(es, "h2T", [128, KC, 512], BF16)
            yacc = sb(es, "yacc", [128, 4, D], F32)
            gus = [sb(es, "gus%d" % i, [128, KC, 2, 256], BF16) for i in range(2)]
            actT = sb(es, "actT", [128, KC, 512], BF16)
            wds = [sb(es, "wds%d" % i, [128, KC, 512], BF16) for i in range(2)]
            g2b = sb(es, "g2b", [128, 2, D], F32)
            gfb = sb(es, "gfb", [128, D], F32)
            dg = sb(es, "dge", [128, 128], F32)
            x1r = sb(es, "x1r", [128, D], F32)
            yo = sb(es, "yo", [128, D], F32)
            bgT = sb(es, "bgT", [128, NE, 32], F32)
            bdf = sb(es, "bdf", [NE, D], F32)
            gtT = sb(es, "gtT", [NE, 128], F32)
            tg = [sb(es, "tg%d" % i, [128, 512], F32) for i in range(2)]
            tsg = [sb(es, "tsg%d" % i, [128, 512], F32) for i in range(2)]
            tu = [sb(es, "tu%d" % i, [128, 512], F32) for i in range(2)]
            ssq = sb(es, "sse", [128, 1], F32)
            rst = sb(es, "rse", [128, 1], F32)

            for e_ in range(NE):
                S.dma("sp", "bgTd", [], ["bgT"], bgT[:, e_, :], b_gu[e_].rearrange("(c p) -> p c", p=128), allow_slow_non_contiguous=True)
            S.dma("sp", "gfbd", [], ["gfb"], gfb[:], g_final.partition_broadcast(128))
            S.dma("sp", "bdfd", [], ["bdf"], bdf[:], b_down)
            for r in range(2):
                for kc in range(KC):
                    S.op("dve", ["adaT", "identf"], ["dge"], nc.vector.tensor_scalar, out=dg[:], in0=identf[:],
                         scalar1=adaT[:, 80 + kc, r:r + 1], scalar2=None, op0=ALU.mult)
                    S.op("pe", ["dge", "ones_f"], ["ps0"], nc.tensor.matmul, ps[0][:, 0:128], lhsT=ones_f[:],
                         rhs=dg[:], start=True, stop=True)
                    S.op("act", ["ps0"], ["g2b"], nc.scalar.copy, out=g2b[:, r, kc * 128:(kc + 1) * 128],
                         in_=ps[0][:, 0:128])
            cnt = dict(gu=0, wd=0, bd=0, t=0)
            sts = [(i, 4, 0) for i in range(NSLOT // 4)] + [(NSLOT // 4, 1, 1)]
            for (sti, nbk, row) in sts:
                TT = 64 if row else nbk * 128
                RW = 64 if row else 128
                q0 = sti * 512
                S.dma("sp", "h2Td", [], ["h2T"], h2T[:, :, 0:TT], H2T[:, :, q0:q0 + TT].rearrange("k p t -> p k t"))
                S.op("dve", [], ["yacc"], nc.vector.memset, yacc[:, 0:nbk, :], 0.0)
                for e in range(NE):
                    for sl in range(8):
                        gu, gun = gus[cnt["gu"] % 2], "gus%d" % (cnt["gu"] % 2)
                        cnt["gu"] += 1
                        for a_ in range(2):
                            S.dma("sp", gun + "h%d" % a_, [("WGU", e, 0), ("WGU", e, 1)], [gun + "h%d" % a_], gu[:, :, a_, :],
                                  WGU[e][:, a_ * 2048 + sl * 256:a_ * 2048 + (sl + 1) * 256].rearrange(
                                      "(k p) c -> p k c", p=128))
                        for i in range(2):
                            ffb = sl * 2 + i
                            for a in range(2):
                                bank = 2 * (ffb % 2) + a
                                bn = "ps%d" % bank
                                for kc in range(KC):
                                    S.op("pe", [gun + "h%d" % a, "h2T"], [bn], nc.tensor.matmul, ps[bank][:, 0:TT],
                                         lhsT=gu[:, kc, a, i * 128:(i + 1) * 128], rhs=h2T[:, kc, 0:TT],
                                         start=(kc == 0), stop=(kc == KC - 1))
                            bg_, bu_ = "ps%d" % (2 * (ffb % 2)), "ps%d" % (2 * (ffb % 2) + 1)
                            pg, pu = ps[2 * (ffb % 2)], ps[2 * (ffb % 2) + 1]
                            k2 = cnt["t"] % 2
                            cnt["t"] += 1
                            g_, s_, u_ = tg[k2], tsg[k2], tu[k2]
                            gn_, sn_, un_ = "tg%d" % k2, "tsg%d" % k2, "tu%d" % k2
                            S.op("dve", [bg_, "bgT"], [gn_], nc.vector.tensor_scalar, out=g_[:, 0:TT], in0=pg[:, 0:TT],
                                 scalar1=bgT[:, e, ffb:ffb + 1], scalar2=7.0, op0=ALU.add, op1=ALU.min)
                            S.op("act", [gn_], [sn_], nc.scalar.activation, out=s_[:, 0:TT], in_=g_[:, 0:TT],
                                 func=AF.Sigmoid, scale=1.702)
                            S.op("dve", [bu_, "bgT"], [un_], nc.vector.tensor_scalar, out=u_[:, 0:TT], in0=pu[:, 0:TT],
                                 scalar1=bgT[:, e, 16 + ffb:17 + ffb], scalar2=7.0, op0=ALU.add, op1=ALU.min)
                            S.op("pool", [un_], [un_], nc.gpsimd.tensor_scalar, out=u_[:, 0:TT], in0=u_[:, 0:TT],
                                 scalar1=-7.0, scalar2=1.0, op0=ALU.max, op1=ALU.add)
                            S.op("pool", [gn_, sn_], [gn_], nc.gpsimd.tensor_tensor, out=g_[:, 0:TT], in0=g_[:, 0:TT],
                                 in1=s_[:, 0:TT], op=ALU.mult)
                            S.op("pool", [gn_, un_], ["actT"], nc.gpsimd.tensor_tensor, out=actT[:, ffb, 0:TT],
                                 in0=g_[:, 0:TT], in1=u_[:, 0:TT], op=ALU.mult)
                    for qd in range(4):
                        wd, wdn = wds[cnt["wd"] % 2], "wds%d" % (cnt["wd"] % 2)
                        cnt["wd"] += 1
                        S.dma("sp", wdn, [("WD", e)], [wdn], wd[:],
                              WD[e][:, qd * 512:(qd + 1) * 512].rearrange("(k p) c -> p k c", p=128))
                        for ts in range(nbk):
                            bank = 4 + (ts % 4)
                            bn = "ps%d" % bank
                            for kc in range(KC):
                                S.op("pe", ["actT", wdn], [bn], nc.tensor.matmul, ps[bank][0:RW, :],
                                     lhsT=actT[:, kc, ts * 128:ts * 128 + RW], rhs=wd[:, kc, :],
                                     start=(kc == 0), stop=(kc == KC - 1))
                            ya = yacc[0:RW, ts, qd * 512:(qd + 1) * 512]
                            S.op("dve", [bn, "Gt", "yacc"], ["yacc"], nc.vector.scalar_tensor_tensor, out=ya,
                                 in0=ps[bank][0:RW, :], scalar=Gt[0:RW, sti * 4 + ts, e:e + 1], in1=ya,
                                 op0=ALU.mult, op1=ALU.add)
                for ts in range(nbk):
                    r0 = q0 + ts * 128
                    S.dma("sp", "x1rd", [], ["x1r"], x1r[:], X1[r0:r0 + 128, :])
                    S.op("pe", ["Gt", "identf"], ["ps0"], nc.tensor.transpose, ps[0][0:NE, 0:128],
                         Gt[:, sti * 4 + ts, :], identf[:])
                    S.op("act", ["ps0"], ["gtT"], nc.scalar.copy, out=gtT[:], in_=ps[0][0:NE, 0:128])
                    for qd in range(4):
                        bank = 1 + (qd % 2)
                        bn = "ps%d" % bank
                        S.op("pe", ["gtT", "bdf"], [bn], nc.tensor.matmul, ps[bank][:, :], lhsT=gtT[:],
                             rhs=bdf[:, qd * 512:(qd + 1) * 512], start=True, stop=True)
                        ya = yacc[:, ts, qd * 512:(qd + 1) * 512]
                        S.op("dve", [bn, "yacc"], ["yacc"], nc.vector.tensor_tensor, out=ya, in0=ps[bank][:, :],
                             in1=ya, op=ALU.add)
                    S.op("dve", ["yacc", "g2b"], ["yo"], nc.vector.tensor_tensor, out=yo[:], in0=yacc[:, ts, :],
                         in1=g2b[:, row, :], op=ALU.mult)
                    S.op("dve", ["yo", "x1r"], ["yo"], nc.vector.tensor_tensor, out=yo[:], in0=yo[:], in1=x1r[:],
                         op=ALU.add)
                    S.op("dve", [], ["sse"], nc.vector.memset, ssq[:], 0.0)
                    S.op("act", ["yo", "sse"], ["x1r", "sse"], nc.scalar.activation,
                         out=x1r[:].bitcast(BF16)[:, 0:D], in_=yo[:], func=AF.Square, accum_out=ssq[:, 0:1])
                    S.op("act", ["sse"], ["rse"], nc.scalar.activation, out=rst[:], in_=ssq[:], func=AF.Sqrt,
                         bias=epsc[:, 0:1], scale=1.0 / D)
                    S.op("dve", ["rse"], ["rse"], nc.vector.reciprocal, out=rst[:], in_=rst[:])
                    S.op("dve", ["yo", "rse", "gfb"], ["yo"], nc.vector.scalar_tensor_tensor, out=yo[:], in0=yo[:],
                         scalar=rst[:, 0:1], in1=gfb[:], op0=ALU.mult, op1=ALU.mult)
                    dst = y_smp if row else y_own[r0:r0 + 128, :]
                    S.dma("sp", "yod", ["yo"], [], dst, yo[:])
        S.mute = False
        stats = S.emit()
        nc._mk_stats = stats
    return nc


def _nt(S, nc, names, t, src_ap, row, hT, col0, hn, g1p, sh1, epsc, identb, psb):
    xt, xb, ssq, rst, junk = t["xt"], t["xb"], t["ssq"], t["rst"], t["junk"]
    _, xtn, xbn, ssn, rsn, jn = names
    S.dma("sp", xtn, [], [xtn], xt[:], src_ap)
    S.op("dve", [], [ssn], nc.vector.memset, ssq[:], 0.0)
    S.op("act", [xtn, ssn], [jn, ssn], nc.scalar.activation, out=junk[:], in_=xt[:], func=AF.Square,
         accum_out=ssq[:, 0:1])
    S.op("act", [ssn], [rsn], nc.scalar.activation, out=rst[:], in_=ssq[:], func=AF.Sqrt,
         bias=epsc[:, 0:1], scale=1.0 / D)
    S.op("dve", [rsn], [rsn], nc.vector.reciprocal, out=rst[:], in_=rst[:])
    S.op("dve", [xtn, rsn], [xbn], nc.vector.tensor_scalar, out=xb[:], in0=xt[:], scalar1=rst[:, 0:1],
         scalar2=None, op0=ALU.mult)
    for half in range(2):
        bank = 6 + half
        bn = "ps%d" % bank
        for k in range(8):
            kc = half * 8 + k
            S.op("pe", [xbn, "identb"], [bn], nc.tensor.transpose, psb(bank)[:, k * 128:(k + 1) * 128],
                 xb[:, kc * 128:(kc + 1) * 128], identb[:])
        dst = hT[:, half * 8:(half + 1) * 8, col0:col0 + 128]
        S.op("dve", [bn, "g1p"], [hn], nc.vector.tensor_tensor, out=dst,
             in0=psb(bank).rearrange("p (k t) -> p k t", k=8),
             in1=g1p[:, row, half * 8:(half + 1) * 8].unsqueeze(2).broadcast_to([128, 8, 128]),
             op=ALU.mult)
        S.op("dve", [hn, "sh1"], [hn], nc.vector.tensor_tensor, out=dst, in0=dst,
             in1=sh1[:, row, half * 8:(half + 1) * 8].unsqueeze(2).broadcast_to([128, 8, 128]),
             op=ALU.add)


def _tables(cfg, j):
    NBS, NSLOT, TQ, SK, PAST = cfg.NBS, cfg.NSLOT, cfg.TQ, cfg.SK, cfg.PAST
    SEQ = NBS * 128
    slopes = 2.0 ** (-(np.arange(1, NH + 1)))
    pos = np.arange(SEQ)
    kaug = np.zeros((NH, 4, SEQ), np.float32)
    kaug_s = np.zeros((NH, 4, SK), np.float32)
    qaug = np.zeros((NH, 4, TQ), np.float32)
    poss = np.arange(SK)
    qpos = np.concatenate([(4 * m + j) * 128 + np.arange(128) for m in range(NSLOT)] + [PAST + np.arange(128)])
    for h in range(NH):
        s = slopes[h]
        kaug[h, 0] = s * 128.0 * (pos // 128); kaug[h, 1] = s * (pos % 128); kaug[h, 2] = 1.0; kaug[h, 3] = 1.0
        kaug_s[h, 0] = s * 128.0 * (poss // 128); kaug_s[h, 1] = s * (poss % 128); kaug_s[h, 2] = 1.0; kaug_s[h, 3] = 1.0
        qaug[h, 0] = 1.0; qaug[h, 1] = 1.0
        qaug[h, 2] = -s * 128.0 * (qpos // 128); qaug[h, 3] = -s * (qpos % 128)
    kk = np.arange(128)[:, None]
    qq = np.arange(128)[None, :]
    gb = np.zeros((128, NH, 4, 128), np.float32)
    gbs = np.zeros((128, NH, 128), np.float32)
    for h in range(NH):
        s = slopes[h]
        for i in range(4):
            if i < j:
                m_ = np.zeros((128, 128), np.float32)
            elif i > j:
                m_ = np.full((128, 128), NEG, np.float32)
            else:
                d = (qq - kk).astype(np.float32)
                m_ = 2.0 * s * np.minimum(d, 0.0)
                m_ = np.where((kk // 64) <= (qq // 64), m_, NEG).astype(np.float32)
            gb[:, h, i, :] = m_
        d = (qq - kk).astype(np.float32)
        m_ = 2.0 * s * np.minimum(d, 0.0)
        m_ = np.where(kk < 64, m_, NEG).astype(np.float32)
        gbs[:, h, :] = m_
    hm = np.ones((1, 128), np.float32)
    if j == 0:
        hm[0, 0:2] = 0.0
    return dict(kaug=kaug, kaug_s=kaug_s, qaug=qaug, gbias=gb.reshape(128, -1), gbias_s=gbs.reshape(128, -1),
                hmask=hm, ident=np.eye(128, dtype=np.float32))


_NC_CACHE = {}
_DEBUG = dict(stop=9, cores=8)


def kernel(x_prompt, x_sample, cache_k, cache_v, state_conv, c_prompt, c_sample,
           g_mix, g_ffn, w_ada, b_ada, w_in, lambda_q1, lambda_k1, lambda_q2, lambda_k2,
           subln_g, conv_w, w_o, w_router, b_router, w_gu, b_gu, w_down, b_down, g_final):
    f = lambda a: np.ascontiguousarray(np.asarray(a, dtype=np.float32))
    x_prompt, x_sample = f(x_prompt), f(x_sample)
    B, SEQ, _ = x_prompt.shape
    NE = int(np.asarray(w_router).shape[-1])
    PAST = int(np.asarray(cache_k).shape[2])
    cfg = Cfg(nbs=SEQ // 128, ne=NE, past=PAST, stop=_DEBUG["stop"])
    key = (cfg.NBS, cfg.NE, cfg.PAST, cfg.stop)
    if key not in _NC_CACHE:
        _NC_CACHE[key] = build(cfg)
    nc = _NC_CACHE[key]
    NSLOT = cfg.NSLOT
    common = dict(
        g_mix=f(g_mix)[0], g_ffn=f(g_ffn)[0], g_final=f(g_final).reshape(1, D),
        w_ada=f(w_ada)[0], b_ada=f(b_ada)[0], w_in=f(w_in)[0],
        lam4=np.stack([f(lambda_q1)[0], f(lambda_k1)[0], f(lambda_q2)[0], f(lambda_k2)[0]]),
        subln_g=f(subln_g)[0].reshape(1, 128), conv_w=f(conv_w)[0], w_o=f(w_o)[0],
        w_router=f(w_router)[0], b_router=f(b_router)[0].reshape(1, NE),
        w_gu=f(w_gu)[0], b_gu=f(b_gu)[0], w_down=f(w_down)[0], b_down=f(b_down)[0])
    ck, cv, sc = f(cache_k)[0], f(cache_v)[0], f(state_conv)[0]
    cp, cs = f(c_prompt), f(c_sample)
    in_maps = []
    own_rows = []
    for c in range(8):
        b, j = c // 4, c % 4
        rows = np.concatenate([(4 * m + j) * 128 + np.arange(128) for m in range(NSLOT)])
        own_rows.append(rows)
        hrows = []
        for m in range(NSLOT):
            p0 = (4 * m + j) * 128
            hrows += [max(p0 - 2, 0), max(p0 - 1, 0)]
        hrows = (hrows + [0] * 128)[:128]
        m_ = dict(common)
        m_.update(_tables(cfg, j))
        m_.update(
            x_all=x_prompt[b], x_own=np.ascontiguousarray(x_prompt[b][rows]),
            x_halo=np.ascontiguousarray(x_prompt[b][np.asarray(hrows)]),
            x_smp=np.concatenate([x_sample[c], np.zeros((64, D), np.float32)], axis=0),
            c2=np.stack([cp[b], cs[c]]),
            cache_k=ck[c].reshape(PAST, AW), cache_v=cv[c].reshape(PAST, AW), state_conv=sc[c])
        in_maps.append(m_)
    ncores = _DEBUG["cores"]
    res = run_bass_kernel_spmd(nc, in_maps[:ncores], core_ids=list(range(ncores)))
    R = list(res.results) + [res.results[0]] * (8 - ncores)
    y_prompt = np.zeros((B, SEQ, D), np.float32)
    k_prompt = np.zeros((1, B, SEQ, NH, 2, 64), np.float32)
    v_prompt = np.zeros((1, B, SEQ, NH, 128), np.float32)
    conv_prompt = np.zeros((1, B, 2, CW), np.float32)
    y_sample = np.zeros((8, 64, D), np.float32)
    k_sample = np.zeros((1, 8, 64, NH, 2, 64), np.float32)
    v_sample = np.zeros((1, 8, 64, NH, 128), np.float32)
    conv_sample = np.zeros((1, 8, 2, CW), np.float32)
    for c in range(8):
        b, j = c // 4, c % 4
        r = R[c]
        rows = own_rows[c]
        y_prompt[b, rows] = r["y_own"]
        k_prompt[0, b, rows] = r["k_own"].reshape(-1, NH, 2, 64)
        v_prompt[0, b, rows] = r["v_own"].reshape(-1, NH, 128)
        if j == 3:
            conv_prompt[0, b] = r["conv_p"]
        y_sample[c] = r["y_smp"][:64]
        k_sample[0, c] = r["k_smp"][:64].reshape(64, NH, 2, 64)
        v_sample[0, c] = r["v_smp"][:64].reshape(64, NH, 128)
        conv_sample[0, c] = r["conv_s"]
    return (y_prompt, y_sample, k_prompt, v_prompt, conv_prompt, k_sample, v_sample, conv_sample)
```

```python
import contextlib
from functools import partial

import numpy as np

import concourse.bass as bass
import concourse.mybir as mybir
from concourse.bass_utils import run_bass_kernel_spmd

F32 = mybir.dt.float32
BF16 = mybir.dt.bfloat16
AF = mybir.ActivationFunctionType
ALU = mybir.AluOpType

D = 2048
KC = 16
NH = 8
AW = 1024
CW = 1024
INC = 6144
DFF = 2048
EPS = 1e-6
LAM_INIT = 0.2
NEG = -30000.0


class Sched:
    def __init__(self, nc, same_eng_sync=True):
        self.nc = nc
        self.ops = []
        self.same_eng_sync = same_eng_sync
        self.mute = False
        self.eng = {"pe": nc.tensor, "act": nc.scalar, "dve": nc.vector,
                    "pool": nc.gpsimd, "sp": nc.sync}

    def op(self, eng, reads, writes, fn, *a, **kw):
        if self.mute:
            return
        self.ops.append(dict(eng=eng, fn=partial(fn, *a, **kw), reads=tuple(reads),
                             writes=tuple(writes), dma=False, lane=None, bar=False, nobar=False))

    def dma(self, q, lane, reads, writes, out, in_, nobar=False, **kw):
        if self.mute:
            return
        fn = partial(self.eng[q].dma_start, out=out, in_=in_, **kw)
        self.ops.append(dict(eng=q, fn=fn, reads=tuple(reads), writes=tuple(writes),
                             dma=True, lane=lane, bar=False, nobar=nobar))

    def barrier(self):
        if self.mute:
            return
        self.ops.append(dict(bar=True))

    def emit(self, final_wait_eng="sp"):
        nc = self.nc
        ops = self.ops
        last_w, readers = {}, {}
        n = len(ops)
        deps = [()] * n
        need = [False] * n
        last_compute, last_dma, pending = {}, {}, {}
        for i, o in enumerate(ops):
            if o["bar"]:
                bd = list(last_compute.values()) + list(last_dma.values())
                for e in self.eng:
                    pending[e] = list(bd)
                continue
            d = set()
            for r in o["reads"]:
                if r in last_w:
                    d.add(last_w[r])
            for w in o["writes"]:
                if w in last_w:
                    d.add(last_w[w])
                d.update(readers.get(w, ()))
            if o["eng"] in pending:
                d.update(pending.pop(o["eng"]))
            d.discard(i)
            keep = []
            for j in d:
                oj = ops[j]
                if (not oj["dma"]) and (not o["dma"]) and oj["eng"] == o["eng"]:
                    if o["eng"] == "pe" or not self.same_eng_sync:
                        continue
                keep.append(j)
                need[j] = True
            deps[i] = keep
            for r in o["reads"]:
                readers.setdefault(r, []).append(i)
            for w in o["writes"]:
                last_w[w] = i
                readers[w] = []
            if o["dma"]:
                if not o["nobar"]:
                    last_dma[o["lane"]] = i
            else:
                last_compute[o["eng"]] = i
        eng_sem, eng_cnt, lane_sem, lane_cnt = {}, {}, {}, {}
        ev = [None] * n
        grp = {}
        for i, o in enumerate(ops):
            if o["bar"]:
                continue
            if o["dma"]:
                ln = o["lane"]
                if ln not in lane_sem:
                    lane_sem[ln] = nc.alloc_semaphore(name="L_" + str(ln))
                    lane_cnt[ln] = 0
                lane_cnt[ln] += 16
                ev[i] = (lane_sem[ln], lane_cnt[ln])
                if o["nobar"]:
                    grp.setdefault(ln, []).append(i)
            elif need[i]:
                e = o["eng"]
                if e not in eng_sem:
                    eng_sem[e] = nc.alloc_semaphore(name="E_" + e)
                    eng_cnt[e] = 0
                eng_cnt[e] += 1
                ev[i] = (eng_sem[e], eng_cnt[e])
        for ln, idxs in grp.items():
            for i in idxs:
                ev[i] = (lane_sem[ln], lane_cnt[ln])
        waited = {}
        nw = 0
        for i, o in enumerate(ops):
            if o["bar"]:
                continue
            e = o["eng"]
            engine = self.eng[e]
            want = {}
            for j in deps[i]:
                s, v = ev[j]
                k = id(s)
                if k not in want or want[k][1] < v:
                    want[k] = (s, v)
            for k, (s, v) in want.items():
                if waited.get((e, k), 0) < v:
                    engine.wait_ge(s, v)
                    waited[(e, k)] = v
                    nw += 1
            inst = o["fn"]()
            if o["dma"]:
                inst.then_inc(lane_sem[o["lane"]], 16)
            elif need[i]:
                inst.then_inc(ev[i][0], 1)
        fe = self.eng[final_wait_eng]
        for ln, s in lane_sem.items():
            fe.wait_ge(s, lane_cnt[ln])
        self.stats = dict(n_ops=n, n_waits=nw, n_lanes=len(lane_sem), n_signals=sum(need))
        return self.stats


class Cfg:
    def __init__(self, nbs=128, ne=32, past=1024, stop=9):
        self.stop = stop
        self.NBS = nbs
        self.NSLOT = nbs // 4
        self.TOWN = self.NSLOT * 128
        self.TQ = self.TOWN + 128
        self.NE = ne
        self.PAST = past
        self.PB = past // 128
        self.SK = past + 128


def build(cfg):
    NBS, NSLOT, TOWN, TQ, NE, PAST, PB, SK = (cfg.NBS, cfg.NSLOT, cfg.TOWN, cfg.TQ, cfg.NE,
                                              cfg.PAST, cfg.PB, cfg.SK)
    SEQ = NBS * 128
    nc = bass.Bass("TRN2", target_bir_lowering=False)
    S = Sched(nc)

    def din(name, shape, dt=F32):
        return nc.dram_tensor(name, list(shape), dt, kind="ExternalInput").ap()

    def dout(name, shape, dt=F32):
        return nc.dram_tensor(name, list(shape), dt, kind="ExternalOutput").ap()

    def dscr(name, shape, dt=BF16):
        return nc.dram_tensor(name, list(shape), dt, kind="Internal").ap()

    x_all = din("x_all", [SEQ, D]); x_own = din("x_own", [TOWN, D])
    x_halo = din("x_halo", [128, D]); x_smp = din("x_smp", [128, D])
    c2 = din("c2", [2, D])
    cache_k = din("cache_k", [PAST, AW]); cache_v = din("cache_v", [PAST, AW])
    state_conv = din("state_conv", [2, CW])
    g_mix = din("g_mix", [D]); g_ffn = din("g_ffn", [D]); g_final = din("g_final", [1, D])
    w_ada = din("w_ada", [D, 6 * D]); b_ada = din("b_ada", [6 * D])
    w_in = din("w_in", [D, INC])
    lam4 = din("lam4", [4, 64]); subln_g = din("subln_g", [1, 128])
    conv_w = din("conv_w", [3, CW]); w_o = din("w_o", [D, D])
    w_router = din("w_router", [D, NE]); b_router = din("b_router", [1, NE])
    w_gu = din("w_gu", [NE, D, 2 * DFF]); b_gu = din("b_gu", [NE, 2 * DFF])
    w_down = din("w_down", [NE, DFF, D]); b_down = din("b_down", [NE, D])
    ident_d = din("ident", [128, 128])
    kaug = din("kaug", [NH, 4, SEQ]); kaug_s = din("kaug_s", [NH, 4, SK])
    qaug = din("qaug", [NH, 4, TQ])
    gbias = din("gbias", [128, NH * 4 * 128]); gbias_s = din("gbias_s", [128, NH * 128])
    hmask = din("hmask", [1, 128])

    y_own = dout("y_own", [TOWN, D]); y_smp = dout("y_smp", [128, D])
    k_own = dout("k_own", [TOWN, AW]); v_own = dout("v_own", [TOWN, AW])
    k_smp = dout("k_smp", [128, AW]); v_smp = dout("v_smp", [128, AW])
    conv_p = dout("conv_p", [2, CW]); conv_s = dout("conv_s", [2, CW])

    KT = dscr("KT", [NH, 2, 64, SEQ]); VA = dscr("VA", [SEQ, NH, 128])
    KTs = dscr("KTs", [NH, 2, 64, SK]); VAs = dscr("VAs", [SK, NH, 128])
    QT = dscr("QT", [NH, 2, 64, TQ])
    AT = dscr("AT", [NH, 128, TQ]); BT = dscr("BT", [8, 128, TQ])
    X1 = dscr("X1", [TQ, D], F32)
    H2T = dscr("H2T", [KC, 128, TQ])
    WIN = dscr("WIN", [D, INC])
    WGU = [dscr("WGU%d" % e, [D, 2 * DFF]) for e in range(NE)]
    WD = [dscr("WD%d" % e, [DFF, D]) for e in range(NE)]

    es_all = contextlib.ExitStack()
    with es_all:
        ps = [es_all.enter_context(nc.psum_tensor("ps%d" % i, [128, 512], F32)) for i in range(8)]

        def psb(i):
            return ps[i][:].bitcast(BF16)

        def sb(es, name, shape, dt):
            return es.enter_context(nc.sbuf_tensor(name, list(shape), dt))

        identf = sb(es_all, "identf", [128, 128], F32)
        identb = sb(es_all, "identb", [128, 128], BF16)
        ones_b = sb(es_all, "ones_b", [128, 128], BF16)
        ones_f = sb(es_all, "ones_f", [128, 128], F32)
        epsc = sb(es_all, "epsc", [128, 1], F32)
        adaT = sb(es_all, "adaT", [128, 96, 2], F32)
        g1p = sb(es_all, "g1p", [128, 2, KC], F32)
        sh1 = sb(es_all, "sh1", [128, 2, KC], F32)
        g2p = sb(es_all, "g2p", [128, 2, KC], F32)
        sh2 = sb(es_all, "sh2", [128, 2, KC], F32)
        gmT = sb(es_all, "gmT", [128, KC], F32)
        gfT = sb(es_all, "gfT", [128, KC], F32)
        lamt = sb(es_all, "lamt", [128, 8], F32)
        Gt = sb(es_all, "Gt", [128, NSLOT + 1, NE], F32)
        cwT = sb(es_all, "cwT", [128, 8, 3], F32)
        ugh = sb(es_all, "ugh", [128, 8, 128], F32)
        ughs = sb(es_all, "ughs", [128, 8, 2], F32)

        S.dma("sp", "c_id", [], ["identf"], identf[:], ident_d)
        S.op("dve", ["identf"], ["identb"], nc.vector.tensor_copy, out=identb[:], in_=identf[:])
        S.op("dve", [], ["ones_b"], nc.vector.memset, ones_b[:], 1.0)
        S.op("dve", [], ["ones_f"], nc.vector.memset, ones_f[:], 1.0)
        S.op("dve", [], ["epsc"], nc.vector.memset, epsc[:], EPS)


        S.dma("sp", "c_gm", [], ["gmT"], gmT[:], g_mix.rearrange("(k p) -> p k", p=128), allow_slow_non_contiguous=True)
        S.dma("sp", "c_gf", [], ["gfT"], gfT[:], g_ffn.rearrange("(k p) -> p k", p=128), allow_slow_non_contiguous=True)
        for tp in range(3):
            S.dma("sp", "c_cw", [], ["cwT"], cwT[:, :, tp], conv_w[tp].rearrange("(cb p) -> p cb", p=128), allow_slow_non_contiguous=True)
        for r_ in range(2):
            S.dma("sp", "c_sc", [], ["ughs"], ughs[:, :, r_], state_conv[r_].rearrange("(cb p) -> p cb", p=128), allow_slow_non_contiguous=True)
        for q in range(3):
            S.dma("pool", "cv_win", [], [("WIN", q)], WIN[:, q * 2048:(q + 1) * 2048],
                  w_in[:, q * 2048:(q + 1) * 2048], nobar=True)

        with contextlib.ExitStack() as es:
            cT = sb(es, "cT", [128, KC, 2], F32)
            scT = sb(es, "scT", [128, KC, 2], F32)
            baT = sb(es, "baT", [128, 96], F32)
            wsl = [sb(es, "wsl%d" % i, [128, KC, 512], F32) for i in range(4)]
            lmb = sb(es, "lmb", [128, 4, 64], F32)
            ljunk = sb(es, "ljunk", [128, 64], F32)
            lsum = sb(es, "lsum", [128, 2], F32)

            for r_ in range(2):
                S.dma("sp", "c_c2", [], ["cT"], cT[:, :, r_], c2[r_].rearrange("(k p) -> p k", p=128), allow_slow_non_contiguous=True)
            S.dma("sp", "c_ba", [], ["baT"], baT[:], b_ada.rearrange("(k p) -> p k", p=128), allow_slow_non_contiguous=True)
            S.dma("sp", "c_lm", [], ["lmb"], lmb[:].rearrange("p a b -> p (a b)"),
                  lam4.rearrange("a b -> (a b)").partition_broadcast(128))
            S.op("act", ["cT"], ["scT"], nc.scalar.activation, out=scT[:], in_=cT[:], func=AF.Silu)
            for cb4 in range(24):
                w = wsl[cb4 % 4]
                wn = "wsl%d" % (cb4 % 4)
                S.dma(("sp", "act")[cb4 % 2], wn, [], [wn], w[:],
                      w_ada[:, cb4 * 512:(cb4 + 1) * 512].rearrange("(k p) c -> p k c", p=128))
                for i in range(4):
                    cb = cb4 * 4 + i
                    for kc in range(KC):
                        S.op("pe", [wn, "scT"], ["ps0"], nc.tensor.matmul, ps[0][:, cb * 2:cb * 2 + 2],
                             lhsT=w[:, kc, i * 128:(i + 1) * 128], rhs=scT[:, kc, :],
                             start=(kc == 0), stop=(kc == KC - 1))
            S.op("dve", ["ps0", "baT"], ["adaT"], nc.vector.tensor_tensor, out=adaT[:],
                 in0=ps[0][:, 0:192].rearrange("p (k r) -> p k r", r=2),
                 in1=baT[:].unsqueeze(2).broadcast_to([128, 96, 2]), op=ALU.add)
            for r in range(2):
                S.op("dve", ["adaT"], ["sh1"], nc.vector.tensor_copy, out=sh1[:, r, :], in_=adaT[:, 0:16, r])
                S.op("dve", ["adaT"], ["sh2"], nc.vector.tensor_copy, out=sh2[:, r, :], in_=adaT[:, 48:64, r])
                S.op("dve", ["adaT", "gmT"], ["g1p"], nc.vector.scalar_tensor_tensor, out=g1p[:, r, :],
                     in0=adaT[:, 16:32, r], scalar=1.0, in1=gmT[:], op0=ALU.add, op1=ALU.mult)
                S.op("dve", ["adaT", "gfT"], ["g2p"], nc.vector.scalar_tensor_tensor, out=g2p[:, r, :],
                     in0=adaT[:, 64:80, r], scalar=1.0, in1=gfT[:], op0=ALU.add, op1=ALU.mult)
            for i in range(2):
                S.op("dve", [], ["lsum"], nc.vector.memset, lsum[:, i:i + 1], 0.0)
                S.op("dve", ["lmb", "lsum"], ["ljunk", "lsum"], nc.vector.scalar_tensor_tensor, out=ljunk[:],
                     in0=lmb[:, 2 * i, :], scalar=1.0, in1=lmb[:, 2 * i + 1, :], op0=ALU.mult, op1=ALU.mult,
                     accum_out=lsum[:, i:i + 1])
            S.op("act", ["lsum"], ["lsum2"], nc.scalar.activation, out=lamt[:, 2:4], in_=lsum[:], func=AF.Exp)
            S.op("dve", ["lsum2"], ["lamt"], nc.vector.tensor_tensor, out=lamt[:, 0:1], in0=lamt[:, 2:3],
                 in1=lamt[:, 3:4], op=ALU.subtract)
            S.op("dve", ["lamt"], ["lamt"], nc.vector.tensor_scalar, out=lamt[:, 0:1], in0=lamt[:, 0:1],
                 scalar1=LAM_INIT, scalar2=None, op0=ALU.add)
            S.op("dve", ["lamt"], ["lamt"], nc.vector.tensor_scalar, out=lamt[:, 1:2], in0=lamt[:, 0:1],
                 scalar1=-1.0, scalar2=None, op0=ALU.mult)
            S.barrier()

        S.mute = cfg.stop < 5
        for e in range(NE):
            for hf in range(2):
                S.dma("pool", "cv_gu", [], [("WGU", e, hf)],
                      WGU[e][hf * 1024:(hf + 1) * 1024, :].rearrange("r (a c) -> r a c", c=2048),
                      w_gu[e, hf * 1024:(hf + 1) * 1024, :].rearrange("r (a c) -> r a c", c=2048), nobar=True)
            S.dma("pool", "cv_d", [], [("WD", e)], WD[e], w_down[e], nobar=True)

        S.mute = cfg.stop < 1
        NTa = dict(g1p=g1p, sh1=sh1, epsc=epsc, identb=identb, psb=psb)
        with contextlib.ExitStack() as es:
            wkv = sb(es, "wkv", [128, KC, 2048], BF16)
            xts = [sb(es, "xa%d" % i, [128, D], F32) for i in range(2)]
            xbs = [sb(es, "xab%d" % i, [128, D], BF16) for i in range(2)]
            ssqs = [sb(es, "ssa%d" % i, [128, 1], F32) for i in range(2)]
            rsts = [sb(es, "rsa%d" % i, [128, 1], F32) for i in range(2)]
            junk = sb(es, "junka", [128, D], BF16)
            hTs = [sb(es, "hTa%d" % i, [128, KC, 512], BF16) for i in range(2)]
            kts = [sb(es, "kta%d" % i, [128, 512], BF16) for i in range(2)]
            vts = [sb(es, "vta%d" % i, [128, 1024], BF16) for i in range(2)]
            S.dma("sp", "wkv", [("WIN", 0), ("WIN", 1)], ["wkv"], wkv[:],
                  WIN[:, 1024:3072].rearrange("(k p) c -> p k c", p=128))
            blk_i = 0
            ev_i = 0
            for t in range(NBS // 4):
                hT = hTs[t % 2]
                hn = "hTa%d" % (t % 2)
                for b in range(4):
                    i2 = blk_i % 2
                    blk_i += 1
                    _nt(S, nc, (None, "xa%d" % i2, "xab%d" % i2, "ssa%d" % i2, "rsa%d" % i2, "junka"),
                        dict(xt=xts[i2], xb=xbs[i2], ssq=ssqs[i2], rst=rsts[i2], junk=junk),
                        x_all[(t * 4 + b) * 128:(t * 4 + b + 1) * 128, :], 0, hT, b * 128, hn, **NTa)
                for h in range(NH):
                    bank = h % 2
                    bn = "ps%d" % bank
                    for kc in range(KC):
                        S.op("pe", ["wkv", hn], [bn], nc.tensor.matmul, ps[bank][:, :],
                             lhsT=wkv[:, kc, h * 128:(h + 1) * 128], rhs=hT[:, kc, :],
                             start=(kc == 0), stop=(kc == KC - 1))
                    kt = kts[ev_i % 2]
                    kn = "kta%d" % (ev_i % 2)
                    ev_i += 1
                    S.op("act", [bn], [kn], nc.scalar.copy, out=kt[:], in_=ps[bank][:, :])
                    for tt in range(2):
                        S.dma("sp", kn + "s%d" % tt, [kn], [], KT[h, tt, :, t * 512:(t + 1) * 512],
                              kt[tt * 64:(tt + 1) * 64, :])
                for b in range(4):
                    vt = vts[b % 2]
                    vn = "vta%d" % (b % 2)
                    for half in range(2):
                        bank = 2 + half
                        bn = "ps%d" % bank
                        for kc in range(KC):
                            S.op("pe", ["wkv", hn], [bn], nc.tensor.matmul, ps[bank][:, :],
                                 lhsT=hT[:, kc, b * 128:(b + 1) * 128],
                                 rhs=wkv[:, kc, 1024 + half * 512:1024 + (half + 1) * 512],
                                 start=(kc == 0), stop=(kc == KC - 1))
                        S.op("dve", [bn], [vn], nc.vector.tensor_copy, out=vt[:, half * 512:(half + 1) * 512],
                             in_=ps[bank][:, :])
                    r0 = (t * 4 + b) * 128
                    S.dma("sp", vn + "s", [vn], [], VA[r0:r0 + 128].rearrange("r h e -> r (h e)"), vt[:])
            S.barrier()

        S.mute = cfg.stop < 2
        with contextlib.ExitStack() as es:
            wsl = [sb(es, "wb%d" % i, [128, KC, 512], BF16) for i in range(3)]
            xts = [sb(es, "xb%d" % i, [128, D], F32) for i in range(2)]
            xbs = [sb(es, "xbb%d" % i, [128, D], BF16) for i in range(2)]
            ssqs = [sb(es, "ssb%d" % i, [128, 1], F32) for i in range(2)]
            rsts = [sb(es, "rsb%d" % i, [128, 1], F32) for i in range(2)]
            junk = sb(es, "junkb", [128, D], BF16)
            hTs = [sb(es, "hTb%d" % i, [128, KC, 512], BF16) for i in range(2)]
            uT = sb(es, "uT", [128, 8, 512], F32)
            gbT = sb(es, "gbT", [128, 8, 512], F32)
            ugx = [sb(es, "ugx%d" % i, [128, 4, 130], F32) for i in range(2)]
            ycv = [sb(es, "ycv%d" % i, [128, 4, 128], F32) for i in range(2)]
            bct = [sb(es, "bct%d" % i, [128, 512], BF16) for i in range(2)]
            qts = [sb(es, "qtb%d" % i, [128, 512], BF16) for i in range(2)]
            kst = [sb(es, "kst%d" % i, [128, 512], F32) for i in range(2)]
            vbs = [sb(es, "vbs%d" % i, [128, 512], BF16) for i in range(2)]
            hmb = sb(es, "hmb", [128, 128], F32)
            cst = sb(es, "cst", [128, 8, 2], F32)
            csts = sb(es, "csts", [128, 8, 2], F32)
            cvb = sb(es, "cvb", [128, 1024], BF16)
            S.dma("sp", "c_hm", [], ["hmb"], hmb[:], hmask.partition_broadcast(128))
            S.mute = cfg.stop < 1.2
            S.dma("pool", "cv_cv", [], [], VAs[0:PAST].rearrange("r h e -> r (h e)"), cache_v)
            for pb in range(PB):
                S.dma("pool", "cvb", [], ["cvb"], cvb[:], cache_k[pb * 128:(pb + 1) * 128, :])
                for h in range(NH):
                    S.op("pe", ["cvb", "identb"], ["ps5"], nc.tensor.transpose, psb(5)[:, h * 128:(h + 1) * 128],
                         cvb[:, h * 128:(h + 1) * 128], identb[:])
                kq = qts[pb % 2]
                kqn = "qtb%d" % (pb % 2)
                for hh in range(2):
                    kq2 = [qts, vbs][hh][pb % 2]
                    kqn2 = ["qtb%d", "vbs%d"][hh] % (pb % 2)
                    S.op("act", ["ps5"], [kqn2], nc.scalar.copy, out=kq2[:], in_=psb(5)[:, hh * 512:(hh + 1) * 512])
                    for tt in range(2):
                        S.dma("sp", kqn2 + "c%d" % tt, [kqn2], [],
                              KTs[hh * 4:(hh + 1) * 4, tt, :, pb * 128:(pb + 1) * 128].rearrange("h d k -> d h k"),
                              kq2[tt * 64:(tt + 1) * 64, :].rearrange("d (h k) -> d h k", h=4))

            tiles = [("halo", x_halo, 1, 0, None)]
            for ti in range(NSLOT // 4):
                tiles.append(("own", x_own, 4, 0, ti))
            tiles.append(("smp", x_smp, 1, 1, None))
            blk_i = 0
            sl_i = 0
            ev = [0]

            def nxt(lst, names, ctr=ev):
                i = ctr[0] % len(lst)
                ctr[0] += 1
                return lst[i], names % i

            for tix, (kind, src, nbk, row, ti) in enumerate(tiles):
                S.mute = cfg.stop < dict(halo=1.4, own=1.6, smp=1.8)[kind]
                NT = nbk * 128
                hT = hTs[tix % 2]
                hn = "hTb%d" % (tix % 2)
                qc0 = TOWN if kind == "smp" else (ti * 512 if kind == "own" else 0)
                for b in range(nbk):
                    i2 = blk_i % 2
                    blk_i += 1
                    r0 = (ti * 512 + b * 128) if kind == "own" else 0
                    _nt(S, nc, (None, "xb%d" % i2, "xbb%d" % i2, "ssb%d" % i2, "rsb%d" % i2, "junkb"),
                        dict(xt=xts[i2], xb=xbs[i2], ssq=ssqs[i2], rst=rsts[i2], junk=junk),
                        src[r0:r0 + 128, :], row, hT, b * 128, hn, **NTa)
                slabs = (6, 7, 10, 11) if kind == "halo" else range(12)
                if kind == "smp" and _DEBUG.get("smp_slabs") is not None:
                    slabs = _DEBUG["smp_slabs"]
                for s in slabs:
                    w = wsl[sl_i % 3]
                    wn = "wb%d" % (sl_i % 3)
                    sl_i += 1
                    S.dma("sp", wn, [("WIN", (s * 512) // 2048)], [wn], w[:],
                          WIN[:, s * 512:(s + 1) * 512].rearrange("(k p) c -> p k c", p=128))
                    fm = s in (0, 1, 6, 7, 8, 9, 10, 11) or (kind == "smp" and s in (2, 3))
                    if fm:
                        for i in range(4):
                            bank = i % 4
                            bn = "ps%d" % bank
                            for kc in range(KC):
                                S.op("pe", [wn, hn], [bn], nc.tensor.matmul, ps[bank][:, 0:NT],
                                     lhsT=w[:, kc, i * 128:(i + 1) * 128], rhs=hT[:, kc, 0:NT],
                                     start=(kc == 0), stop=(kc == KC - 1))
                            if s in (0, 1):
                                h = 4 * s + i
                                qt, qn = nxt(qts, "qtb%d")
                                S.op("act", [bn], [qn], nc.scalar.activation, out=qt[:, 0:NT], in_=ps[bank][:, 0:NT],
                                     func=AF.Copy, scale=0.125)
                                for tt in range(2):
                                    S.dma("sp", qn + "q%d" % tt, [qn], [], QT[h, tt, :, qc0:qc0 + NT],
                                          qt[tt * 64:(tt + 1) * 64, 0:NT])
                            elif s in (2, 3):
                                h = 4 * (s - 2) + i
                                qt, qn = nxt(qts, "qtb%d")
                                S.op("act", [bn], [qn], nc.scalar.copy, out=qt[:, 0:NT], in_=ps[bank][:, 0:NT])
                                for tt in range(2):
                                    S.dma("sp", qn + "q%d" % tt, [qn], [], KTs[h, tt, :, PAST:PAST + 128],
                                          qt[tt * 64:(tt + 1) * 64, 0:128])
                            elif s in (6, 7):
                                cb = 4 * (s - 6) + i
                                S.op("act", [bn], ["uT"], nc.scalar.copy, out=uT[:, cb, 0:NT], in_=ps[bank][:, 0:NT])
                            elif s in (8, 9):
                                cb = 4 * (s - 8) + i
                                S.op("act", [bn], ["gbT"], nc.scalar.copy, out=gbT[:, cb, 0:NT], in_=ps[bank][:, 0:NT])
                            else:
                                cb = 4 * (s - 10) + i
                                if kind == "halo":
                                    S.op("dve", [bn, "uT"], ["ugh"], nc.vector.tensor_tensor, out=ugh[:, cb, :],
                                         in0=ps[bank][:, 0:128], in1=uT[:, cb, 0:128], op=ALU.mult)
                                    S.op("dve", ["ugh", "hmb"], ["ugh"], nc.vector.tensor_tensor, out=ugh[:, cb, :],
                                         in0=ugh[:, cb, :], in1=hmb[:], op=ALU.mult)
                                    continue
                                ux, uxn = nxt(ugx, "ugx%d")
                                yc, ycn = nxt(ycv, "ycv%d")
                                bc, bcn = nxt(bct, "bct%d")
                                S.op("dve", [bn, "uT"], [uxn], nc.vector.tensor_tensor, out=ux[:, 0:nbk, 2:130],
                                     in0=ps[bank][:, 0:NT].rearrange("p (b t) -> p b t", t=128),
                                     in1=uT[:, cb, 0:NT].rearrange("p (b t) -> p b t", t=128), op=ALU.mult)
                                if kind == "own":
                                    S.op("dve", ["ugh"], [uxn], nc.vector.tensor_copy, out=ux[:, 0:4, 0:2],
                                         in_=ugh[:, cb, ti * 8:ti * 8 + 8].rearrange("p (b r) -> p b r", r=2))
                                else:
                                    S.op("dve", ["ughs"], [uxn], nc.vector.tensor_copy, out=ux[:, 0, 0:2],
                                         in_=ughs[:, cb, :])
                                S.op("dve", [uxn, "cwT"], [ycn], nc.vector.tensor_scalar, out=yc[:, 0:nbk, :],
                                     in0=ux[:, 0:nbk, 2:130], scalar1=cwT[:, cb, 2:3], scalar2=None, op0=ALU.mult)
                                for tap in (1, 0):
                                    S.op("dve", [uxn, "cwT", ycn], [ycn], nc.vector.scalar_tensor_tensor,
                                         out=yc[:, 0:nbk, :], in0=ux[:, 0:nbk, tap:tap + 128],
                                         scalar=cwT[:, cb, tap:tap + 1], in1=yc[:, 0:nbk, :],
                                         op0=ALU.mult, op1=ALU.add)
                                S.op("dve", [ycn, "gbT"], [bcn], nc.vector.tensor_tensor,
                                     out=bc[:, 0:NT].rearrange("p (b t) -> p b t", t=128), in0=yc[:, 0:nbk, :],
                                     in1=gbT[:, cb, 0:NT].rearrange("p (b t) -> p b t", t=128), op=ALU.mult)
                                S.dma("sp", bcn + "s", [bcn], [], BT[cb, :, qc0:qc0 + NT], bc[:, 0:NT])
                                if kind == "own" and ti == NSLOT // 4 - 1:
                                    S.op("dve", [uxn], ["cst"], nc.vector.tensor_copy, out=cst[:, cb, :],
                                         in_=ux[:, 3, 128:130])
                                if kind == "smp":
                                    S.op("dve", [uxn], ["csts"], nc.vector.tensor_copy, out=csts[:, cb, :],
                                         in_=ux[:, 0, 64:66])
                    if s in (2, 3, 4, 5):
                        for b in range(nbk):
                            bank = _DEBUG.get("tm_bank", 4) + (b % 2)
                            bn = "ps%d" % bank
                            for kc in range(KC):
                                S.op("pe", [wn, hn], [bn], nc.tensor.matmul, ps[bank][:, :],
                                     lhsT=hT[:, kc, b * 128:(b + 1) * 128], rhs=w[:, kc, :],
                                     start=(kc == 0), stop=(kc == KC - 1))
                            ks, ksn = nxt(kst, "kst%d")
                            sk_ = _DEBUG.get("tm_skip", ())
                            if "ks" not in sk_:
                                S.op("dve", [bn], [ksn], nc.vector.tensor_copy, out=ks[:], in_=ps[bank][:, :])
                            c0 = (s % 2) * 512
                            if kind == "own":
                                r0 = ti * 512 + b * 128
                                dst = (k_own if s < 4 else v_own)[r0:r0 + 128, c0:c0 + 512]
                            else:
                                dst = (k_smp if s < 4 else v_smp)[:, c0:c0 + 512]
                            if "ksdma" not in sk_:
                                S.dma("sp", ksn + "s", [ksn], [], dst, ks[:])
                            if kind == "smp" and s in (4, 5):
                                vb, vbn = nxt(vbs, "vbs%d")
                                if "vb" not in sk_:
                                    S.op("act", [ksn], [vbn], nc.scalar.copy, out=vb[:], in_=ks[:])
                                if "vbdma" not in sk_:
                                    S.dma("sp", vbn + "s", [vbn], [],
                                          VAs[PAST:PAST + 128, (s - 4) * 4:(s - 4) * 4 + 4, :].rearrange("r h e -> r (h e)"),
                                          vb[:])

            S.mute = cfg.stop < 2
            for r_ in range(2):
                S.dma("sp", "cst_o%d" % r_, ["cst"], [], conv_p[r_].rearrange("(cb p) -> p cb", p=128), cst[:, :, r_], allow_slow_non_contiguous=True)
                S.dma("sp", "csts_o%d" % r_, ["csts"], [], conv_s[r_].rearrange("(cb p) -> p cb", p=128), csts[:, :, r_], allow_slow_non_contiguous=True)
            S.barrier()

        S.mute = cfg.stop < 3
        with contextlib.ExitStack() as es:
            KA = sb(es, "KA", [68, 2, SEQ], BF16)
            VT = sb(es, "VT", [128, NBS, 130], BF16)
            QA = sb(es, "QA", [68, 2, TQ], BF16)
            KAs = sb(es, "KAs", [68, 2, SK], BF16)
            VTs = sb(es, "VTs", [128, PB + 1, 130], BF16)
            GB = sb(es, "GB", [128, NH * 4 * 128], F32)
            GBs = sb(es, "GBs", [128, NH * 128], F32)
            PTs = [sb(es, "PT%d" % i, [128, 512], BF16) for i in range(3)]
            tmS = [sb(es, "tmS%d" % i, [128, 512], F32) for i in range(2)]
            sgb = sb(es, "sgb", [128, 128], F32)
            fin = [sb(es, "fin%d" % i, [128, 8], F32) for i in range(2)]
            otm = [sb(es, "otm%d" % i, [128, 128], F32) for i in range(2)]
            ocm = [sb(es, "ocm%d" % i, [128, 128], F32) for i in range(2)]
            ojk = sb(es, "ojk", [128, 128], F32)
            abf = [sb(es, "abf%d" % i, [128, 128], BF16) for i in range(2)]
            atT = [sb(es, "atT%d" % i, [128, 128], BF16) for i in range(2)]
            S.dma("sp", "c_gb", [], ["GB"], GB[:], gbias)
            S.dma("sp", "c_gbs", [], ["GBs"], GBs[:], gbias_s)
            S.dma("sp", "c_sg", [], ["sgb"], sgb[:], subln_g.partition_broadcast(128))
            S.op("dve", ["sgb"], ["sgb"], nc.vector.tensor_scalar, out=sgb[:], in0=sgb[:], scalar1=1.0 - LAM_INIT,
                 scalar2=None, op0=ALU.mult)
            S.op("dve", [], ["VT"], nc.vector.memset, VT[:, :, 128:130], 1.0)
            S.op("dve", [], ["VTs"], nc.vector.memset, VTs[:, :, 128:130], 1.0)
            ctr = dict(pt=0, s=0, f=0)

            def attend(h, qcol, Kt, Kn, Vt_, Vn, plain_groups, corr_blocks, corr_ap, slot_id):
                fi = ctr["f"] % 2
                ctr["f"] += 1
                obank = [4 + 0, 4 + 1] if fi == 0 else [6, 7]
                for t in range(2):
                    ob = obank[t]
                    obn = "ps%d" % ob
                    groups = [(g, False) for g in plain_groups] + [(corr_blocks, True)]
                    first = True
                    for gi, (blks, is_corr) in enumerate(groups):
                        sbank = ctr["s"] % 4
                        ctr["s"] += 1
                        sbn = "ps%d" % sbank
                        nb = len(blks)
                        for i, kb in enumerate(blks):
                            S.op("pe", [Kn, "QA"], [sbn], nc.tensor.matmul, ps[sbank][:, i * 128:(i + 1) * 128],
                                 lhsT=Kt[0:68, t, kb * 128:(kb + 1) * 128], rhs=QA[0:68, t, qcol:qcol + 128],
                                 start=True, stop=True)
                        pt = PTs[ctr["pt"] % 3]
                        ptn = "PT%d" % (ctr["pt"] % 3)
                        ctr["pt"] += 1
                        if is_corr:
                            tm = tmS[gi % 2]
                            tmn = "tmS%d" % (gi % 2)
                            S.op("dve", [sbn, "GB", "GBs"], [tmn], nc.vector.tensor_tensor, out=tm[:, 0:nb * 128],
                                 in0=ps[sbank][:, 0:nb * 128], in1=corr_ap, op=ALU.add)
                            S.op("act", [tmn], [ptn], nc.scalar.activation, out=pt[:, 0:nb * 128],
                                 in_=tm[:, 0:nb * 128], func=AF.Exp)
                        else:
                            S.op("act", [sbn], [ptn], nc.scalar.activation, out=pt[:, 0:nb * 128],
                                 in_=ps[sbank][:, 0:nb * 128], func=AF.Exp)
                        for i, kb in enumerate(blks):
                            last = (gi == len(groups) - 1) and (i == nb - 1)
                            S.op("pe", [ptn, Vn], [obn], nc.tensor.matmul, ps[ob][:, 0:129],
                                 lhsT=pt[:, i * 128:(i + 1) * 128], rhs=Vt_[:, kb, 0:129],
                                 start=first, stop=last)
                            first = False
                f = fin[fi]
                fn_ = "fin%d" % fi
                o0, o1 = ps[obank[0]], ps[obank[1]]
                o0n, o1n = "ps%d" % obank[0], "ps%d" % obank[1]
                S.op("dve", [o0n], [fn_], nc.vector.reciprocal, out=f[:, 0:1], in_=o0[:, 128:129])
                S.op("dve", [o1n], [fn_], nc.vector.reciprocal, out=f[:, 1:2], in_=o1[:, 128:129])
                S.op("dve", [fn_, "lamt"], [fn_], nc.vector.tensor_tensor, out=f[:, 1:2], in0=f[:, 1:2],
                     in1=lamt[:, 1:2], op=ALU.mult)
                ot, otn = otm[fi], "otm%d" % fi
                oc, ocn = ocm[fi], "ocm%d" % fi
                S.op("dve", [o1n, fn_], [otn], nc.vector.tensor_scalar, out=ot[:], in0=o1[:, 0:128],
                     scalar1=f[:, 1:2], scalar2=None, op0=ALU.mult)
                S.op("dve", [o0n, fn_, otn], [ocn], nc.vector.scalar_tensor_tensor, out=oc[:], in0=o0[:, 0:128],
                     scalar=f[:, 0:1], in1=ot[:], op0=ALU.mult, op1=ALU.add)
                S.op("dve", [], [fn_], nc.vector.memset, f[:, 2:3], 0.0)
                S.op("dve", [ocn, fn_], ["ojk", fn_], nc.vector.scalar_tensor_tensor, out=ojk[:], in0=oc[:],
                     scalar=1.0, in1=oc[:], op0=ALU.mult, op1=ALU.mult, accum_out=f[:, 2:3])
                S.op("act", [fn_], [fn_], nc.scalar.activation, out=f[:, 3:4], in_=f[:, 2:3], func=AF.Ln,
                     bias=epsc[:, 0:1], scale=1.0 / 128)
                S.op("act", [fn_], [fn_], nc.scalar.activation, out=f[:, 4:5], in_=f[:, 3:4], func=AF.Exp,
                     scale=-0.5)
                ab, abn = abf[fi], "abf%d" % fi
                S.op("dve", [ocn, fn_, "sgb"], [abn], nc.vector.scalar_tensor_tensor, out=ab[:], in0=oc[:],
                     scalar=f[:, 4:5], in1=sgb[:], op0=ALU.mult, op1=ALU.mult)
                tb = obank[0]
                tbn = "ps%d" % tb
                S.op("pe", [abn, "identb"], [tbn], nc.tensor.transpose, psb(tb)[:, 512:640], ab[:], identb[:])
                at, atn = atT[fi], "atT%d" % fi
                S.op("act", [tbn], [atn], nc.scalar.copy, out=at[:], in_=psb(tb)[:, 512:640])
                S.dma("sp", atn + "s", [atn], [], AT[h, :, qcol:qcol + 128], at[:])

            for h in range(NH):
                S.dma("sp", "KAd", [], ["KA"], KA[0:64, :, :], KT[h].rearrange("t d k -> d t k"))
                S.dma("sp", "QAd", [], ["QA"], QA[0:64, :, :], QT[h].rearrange("t d k -> d t k"))
                S.dma("sp", "KAsd", [], ["KAs"], KAs[0:64, :, :], KTs[h].rearrange("t d k -> d t k"))
                for t in range(2):
                    S.dma("pool", "KAa%d" % t, [], ["KA"], KA[64:68, t, :], kaug[h])
                    S.dma("pool", "QAa%d" % t, [], ["QA"], QA[64:68, t, :], qaug[h])
                    S.dma("pool", "KAsa%d" % t, [], ["KAs"], KAs[64:68, t, :], kaug_s[h])
                S.dma("sp", "VTd", [], ["VT"], VT[:, :, 0:128], VA[:, h, :].rearrange("(kb p) e -> p kb e", p=128))
                S.dma("sp", "VTsd", [], ["VTs"], VTs[:, :, 0:128], VAs[:, h, :].rearrange("(kb p) e -> p kb e", p=128))
                for m in range(NSLOT):
                    plain = [[4 * g + i for i in range(4)] for g in range(m)]
                    corr = [4 * m + i for i in range(4)]
                    attend(h, m * 128, KA, "KA", VT, "VT", plain, corr, GB[:, h * 512:(h + 1) * 512], m)
                plain = [[4 * g + i for i in range(4)] for g in range(PB // 4)]
                attend(h, TOWN, KAs, "KAs", VTs, "VTs", plain, [PB], GBs[:, h * 128:(h + 1) * 128], NSLOT)
            S.barrier()

        S.mute = cfg.stop < 4
        with contextlib.ExitStack() as es:
            WO = sb(es, "WO", [128, KC, D], BF16)
            WR = sb(es, "WR", [128, KC, NE], F32)
            brb = sb(es, "brb", [128, NE], F32)
            g1b = sb(es, "g1b", [128, 2, D], F32)
            dg = sb(es, "dg", [128, 128], F32)
            catT = [sb(es, "catT%d" % i, [128, KC, 128], BF16) for i in range(2)]
            xts = [sb(es, "xd%d" % i, [128, D], F32) for i in range(2)]
            x1s = [sb(es, "x1d%d" % i, [128, D], F32) for i in range(2)]
            xn2 = sb(es, "xn2", [128, D], F32)
            junk = sb(es, "junkd", [128, D], BF16)
            h32 = sb(es, "h32", [128, KC, 128], F32)
            h2b = [sb(es, "h2b%d" % i, [128, KC, 128], BF16) for i in range(2)]
            ssq = sb(es, "ssd", [128, 1], F32)
            rst = sb(es, "rsd", [128, 1], F32)
            lg = sb(es, "lg", [128, NE], F32)
            t8 = sb(es, "t8", [128, 8], F32)
            msk = sb(es, "msk", [128, NE], F32)
            exl = sb(es, "exl", [128, NE], F32)
            den = sb(es, "den", [128, 2], F32)
            S.dma("pool", "WOd", [], ["WO"], WO[:], w_o.rearrange("(k p) c -> p k c", p=128))
            S.dma("sp", "WRd", [], ["WR"], WR[:], w_router.rearrange("(k p) c -> p k c", p=128))
            S.dma("sp", "brd", [], ["brb"], brb[:], b_router.partition_broadcast(128))
            for r in range(2):
                for kc in range(KC):
                    S.op("dve", ["adaT", "identf"], ["dg"], nc.vector.tensor_scalar, out=dg[:], in0=identf[:],
                         scalar1=adaT[:, 32 + kc, r:r + 1], scalar2=None, op0=ALU.mult)
                    S.op("pe", ["dg", "ones_f"], ["ps0"], nc.tensor.matmul, ps[0][:, 0:128], lhsT=ones_f[:],
                         rhs=dg[:], start=True, stop=True)
                    S.op("act", ["ps0"], ["g1b"], nc.scalar.copy, out=g1b[:, r, kc * 128:(kc + 1) * 128],
                         in_=ps[0][:, 0:128])
            for blk in range(NSLOT + 1):
                row = 1 if blk == NSLOT else 0
                q0 = blk * 128
                ct, ctn = catT[blk % 2], "catT%d" % (blk % 2)
                xt, xtn = xts[blk % 2], "xd%d" % (blk % 2)
                x1, x1n = x1s[blk % 2], "x1d%d" % (blk % 2)
                hb, hbn = h2b[blk % 2], "h2b%d" % (blk % 2)
                S.dma("sp", ctn + "a", [], [ctn], ct[:, 0:8, :], AT[:, :, q0:q0 + 128].rearrange("h e q -> e h q"))
                S.dma("sp", ctn + "b", [], [ctn], ct[:, 8:16, :], BT[:, :, q0:q0 + 128].rearrange("h e q -> e h q"))
                S.dma("sp", xtn, [], [xtn], xt[:], (x_smp if row else x_own[q0:q0 + 128, :]))
                for nb_ in range(4):
                    bank = nb_ % 4
                    bn = "ps%d" % bank
                    for kc in range(KC):
                        S.op("pe", [ctn, "WO"], [bn], nc.tensor.matmul, ps[bank][:, :], lhsT=ct[:, kc, :],
                             rhs=WO[:, kc, nb_ * 512:(nb_ + 1) * 512], start=(kc == 0), stop=(kc == KC - 1))
                    S.op("dve", [bn, "g1b"], [x1n], nc.vector.tensor_tensor, out=x1[:, nb_ * 512:(nb_ + 1) * 512],
                         in0=ps[bank][:, :], in1=g1b[:, row, nb_ * 512:(nb_ + 1) * 512], op=ALU.mult)
                S.op("dve", [x1n, xtn], [x1n], nc.vector.tensor_tensor, out=x1[:], in0=x1[:], in1=xt[:], op=ALU.add)
                S.dma("sp", x1n + "s", [x1n], [], X1[q0:q0 + 128, :], x1[:])
                S.op("dve", [], ["ssd"], nc.vector.memset, ssq[:], 0.0)
                S.op("act", [x1n, "ssd"], ["junkd", "ssd"], nc.scalar.activation, out=junk[:], in_=x1[:],
                     func=AF.Square, accum_out=ssq[:, 0:1])
                S.op("act", ["ssd"], ["rsd"], nc.scalar.activation, out=rst[:], in_=ssq[:], func=AF.Sqrt,
                     bias=epsc[:, 0:1], scale=1.0 / D)
                S.op("dve", ["rsd"], ["rsd"], nc.vector.reciprocal, out=rst[:], in_=rst[:])
                S.op("dve", [x1n, "rsd"], ["xn2"], nc.vector.tensor_scalar, out=xn2[:], in0=x1[:],
                     scalar1=rst[:, 0:1], scalar2=None, op0=ALU.mult)
                for q4 in range(4):
                    bank = 4 + (q4 % 2)
                    bn = "ps%d" % bank
                    for k in range(4):
                        kc = q4 * 4 + k
                        S.op("pe", ["xn2", "identf"], [bn], nc.tensor.transpose, ps[bank][:, k * 128:(k + 1) * 128],
                             xn2[:, kc * 128:(kc + 1) * 128], identf[:])
                    dst = h32[:, q4 * 4:(q4 + 1) * 4, :]
                    S.op("dve", [bn, "g2p"], ["h32"], nc.vector.tensor_tensor, out=dst,
                         in0=ps[bank][:, :].rearrange("p (k t) -> p k t", k=4),
                         in1=g2p[:, row, q4 * 4:(q4 + 1) * 4].unsqueeze(2).broadcast_to([128, 4, 128]), op=ALU.mult)
                    S.op("dve", ["h32", "sh2"], ["h32"], nc.vector.tensor_tensor, out=dst, in0=dst,
                         in1=sh2[:, row, q4 * 4:(q4 + 1) * 4].unsqueeze(2).broadcast_to([128, 4, 128]), op=ALU.add)
                S.op("act", ["h32"], [hbn], nc.scalar.copy, out=hb[:], in_=h32[:])
                S.dma("sp", hbn + "s", [hbn], [], H2T[:, :, q0:q0 + 128].rearrange("k p t -> p k t"), hb[:])
                for kc in range(KC):
                    S.op("pe", ["h32", "WR"], ["ps6"], nc.tensor.matmul, ps[6][:, 0:NE], lhsT=h32[:, kc, :],
                         rhs=WR[:, kc, :], start=(kc == 0), stop=(kc == KC - 1))
                S.op("dve", ["ps6", "brb"], ["lg"], nc.vector.tensor_tensor, out=lg[:], in0=ps[6][:, 0:NE],
                     in1=brb[:], op=ALU.add)
                S.op("dve", ["lg"], ["t8"], nc.vector.max, out=t8[:], in_=lg[:])
                S.op("dve", ["lg", "t8"], ["msk"], nc.vector.tensor_scalar, out=msk[:], in0=lg[:],
                     scalar1=t8[:, 3:4], scalar2=None, op0=ALU.is_ge)
                S.op("dve", ["t8"], ["den"], nc.vector.tensor_scalar, out=den[:, 1:2], in0=t8[:, 0:1],
                     scalar1=-1.0, scalar2=None, op0=ALU.mult)
                S.op("act", ["lg", "den"], ["exl"], nc.scalar.activation, out=exl[:], in_=lg[:], func=AF.Exp,
                     bias=den[:, 1:2], scale=1.0)
                S.op("dve", [], ["den0"], nc.vector.memset, den[:, 0:1], 0.0)
                S.op("dve", ["exl", "msk", "den0"], ["exl", "den0"], nc.vector.scalar_tensor_tensor, out=exl[:],
                     in0=exl[:], scalar=1.0, in1=msk[:], op0=ALU.mult, op1=ALU.mult, accum_out=den[:, 0:1])
                S.op("dve", ["den0"], ["den0"], nc.vector.reciprocal, out=den[:, 0:1], in_=den[:, 0:1])
                S.op("dve", ["exl", "den0"], ["Gt"], nc.vector.tensor_scalar, out=Gt[:, blk, :], in0=exl[:],
                     scalar1=den[:, 0:1], scalar2=None, op0=ALU.mult)
            S.barrier()

        S.mute = cfg.stop < 5
        with contextlib.ExitStack() as es:
            h2T = sb(es, "h2T", [128, KC, 512], BF16)
            yacc = sb(es, "yacc", [128, 4, D], F32)
            gus = [sb(es, "gus%d" % i, [128, KC, 2, 256], BF16) for i in range(2)]
            actT = sb(es, "actT", [128, KC, 512], BF16)
            wds = [sb(es, "wds%d" % i, [128, KC, 512], BF16) for i in range(2)]
            g2b = sb(es, "g2b", [128, 2, D], F32)
            gfb = sb(es, "gfb", [128, D], F32)
            dg = sb(es, "dge", [128, 128], F32)
            x1r = sb(es, "x1r", [128, D], F32)
            yo = sb(es, "yo", [128, D], F32)
            bgT = sb(es, "bgT", [128, NE, 32], F32)
            bdf = sb(es, "bdf", [NE, D], F32)
            gtT = sb(es, "gtT", [NE, 128], F32)
            tg = [sb(es, "tg%d" % i, [128, 512], F32) for i in range(2)]
            tsg = [sb(es, "tsg%d" % i, [128, 512], F32) for i in range(2)]
            tu = [sb(es, "tu%d" % i, [128, 512], F32) for i in range(2)]
            ssq = sb(es, "sse", [128, 1], F32)
            rst = sb(es, "rse", [128, 1], F32)

            for e_ in range(NE):
                S.dma("sp", "bgTd", [], ["bgT"], bgT[:, e_, :], b_gu[e_].rearrange("(c p) -> p c", p=128), allow_slow_non_contiguous=True)
            S.dma("sp", "gfbd", [], ["gfb"], gfb[:], g_final.partition_broadcast(128))
            S.dma("sp", "bdfd", [], ["bdf"], bdf[:], b_down)
            for r in range(2):
                for kc in range(KC):
                    S.op("dve", ["adaT", "identf"], ["dge"], nc.vector.tensor_scalar, out=dg[:], in0=identf[:],
                         scalar1=adaT[:, 80 + kc, r:r + 1], scalar2=None, op0=ALU.mult)
                    S.op("pe", ["dge", "ones_f"], ["ps0"], nc.tensor.matmul, ps[0][:, 0:128], lhsT=ones_f[:],
                         rhs=dg[:], start=True, stop=True)
                    S.op("act", ["ps0"], ["g2b"], nc.scalar.copy, out=g2b[:, r, kc * 128:(kc + 1) * 128],
                         in_=ps[0][:, 0:128])
            cnt = dict(gu=0, wd=0, bd=0, t=0)
            sts = [(i, 4, 0) for i in range(NSLOT // 4)] + [(NSLOT // 4, 1, 1)]
            for (sti, nbk, row) in sts:
                TT = 64 if row else nbk * 128
                RW = 64 if row else 128
                q0 = sti * 512
                S.dma("sp", "h2Td", [], ["h2T"], h2T[:, :, 0:TT], H2T[:, :, q0:q0 + TT].rearrange("k p t -> p k t"))
                S.op("dve", [], ["yacc"], nc.vector.memset, yacc[:, 0:nbk, :], 0.0)
                for e in range(NE):
                    for sl in range(8):
                        gu, gun = gus[cnt["gu"] % 2], "gus%d" % (cnt["gu"] % 2)
                        cnt["gu"] += 1
                        for a_ in range(2):
                            S.dma("sp", gun + "h%d" % a_, [("WGU", e, 0), ("WGU", e, 1)], [gun + "h%d" % a_], gu[:, :, a_, :],
                                  WGU[e][:, a_ * 2048 + sl * 256:a_ * 2048 + (sl + 1) * 256].rearrange(
                                      "(k p) c -> p k c", p=128))
                        for i in range(2):
                            ffb = sl * 2 + i
                            for a in range(2):
                                bank = 2 * (ffb % 2) + a
                                bn = "ps%d" % bank
                                for kc in range(KC):
                                    S.op("pe", [gun + "h%d" % a, "h2T"], [bn], nc.tensor.matmul, ps[bank][:, 0:TT],
                                         lhsT=gu[:, kc, a, i * 128:(i + 1) * 128], rhs=h2T[:, kc, 0:TT],
                                         start=(kc == 0), stop=(kc == KC - 1))
                            bg_, bu_ = "ps%d" % (2 * (ffb % 2)), "ps%d" % (2 * (ffb % 2) + 1)
                            pg, pu = ps[2 * (ffb % 2)], ps[2 * (ffb % 2) + 1]
                            k2 = cnt["t"] % 2
                            cnt["t"] += 1
                            g_, s_, u_ = tg[k2], tsg[k2], tu[k2]
                            gn_, sn_, un_ = "tg%d" % k2, "tsg%d" % k2, "tu%d" % k2
                            S.op("dve", [bg_, "bgT"], [gn_], nc.vector.tensor_scalar, out=g_[:, 0:TT], in0=pg[:, 0:TT],
                                 scalar1=bgT[:, e, ffb:ffb + 1], scalar2=7.0, op0=ALU.add, op1=ALU.min)
                            S.op("act", [gn_], [sn_], nc.scalar.activation, out=s_[:, 0:TT], in_=g_[:, 0:TT],
                                 func=AF.Silu, scale=1.702)
                            S.op("dve", [bu_, "bgT"], [un_], nc.vector.tensor_scalar, out=u_[:, 0:TT], in0=pu[:, 0:TT],
                                 scalar1=bgT[:, e, 16 + ffb:17 + ffb], scalar2=7.0, op0=ALU.add, op1=ALU.min)
                            S.op("pool", [un_], [un_], nc.gpsimd.tensor_scalar, out=u_[:, 0:TT], in0=u_[:, 0:TT],
                                 scalar1=-7.0, scalar2=1.0, op0=ALU.max, op1=ALU.add)
                            S.op("dve", [sn_, un_], ["actT"], nc.vector.scalar_tensor_tensor, out=actT[:, ffb, 0:TT],
                                 in0=s_[:, 0:TT], scalar=1.0 / 1.702, in1=u_[:, 0:TT], op0=ALU.mult, op1=ALU.mult)
                    for qd in range(4):
                        wd, wdn = wds[cnt["wd"] % 2], "wds%d" % (cnt["wd"] % 2)
                        cnt["wd"] += 1
                        S.dma("sp", wdn, [("WD", e)], [wdn], wd[:],
                              WD[e][:, qd * 512:(qd + 1) * 512].rearrange("(k p) c -> p k c", p=128))
                        for ts in range(nbk):
                            bank = 4 + (ts % 4)
                            bn = "ps%d" % bank
                            for kc in range(KC):
                                S.op("pe", ["actT", wdn], [bn], nc.tensor.matmul, ps[bank][0:RW, :],
                                     lhsT=actT[:, kc, ts * 128:ts * 128 + RW], rhs=wd[:, kc, :],
                                     start=(kc == 0), stop=(kc == KC - 1))
                            ya = yacc[0:RW, ts, qd * 512:(qd + 1) * 512]
                            S.op("dve", [bn, "Gt", "yacc"], ["yacc"], nc.vector.scalar_tensor_tensor, out=ya,
                                 in0=ps[bank][0:RW, :], scalar=Gt[0:RW, sti * 4 + ts, e:e + 1], in1=ya,
                                 op0=ALU.mult, op1=ALU.add)
                for ts in range(nbk):
                    r0 = q0 + ts * 128
                    S.dma("sp", "x1rd", [], ["x1r"], x1r[:], X1[r0:r0 + 128, :])
                    S.op("pe", ["Gt", "identf"], ["ps0"], nc.tensor.transpose, ps[0][0:NE, 0:128],
                         Gt[:, sti * 4 + ts, :], identf[:])
                    S.op("act", ["ps0"], ["gtT"], nc.scalar.copy, out=gtT[:], in_=ps[0][0:NE, 0:128])
                    for qd in range(4):
                        bank = 1 + (qd % 2)
                        bn = "ps%d" % bank
                        S.op("pe", ["gtT", "bdf"], [bn], nc.tensor.matmul, ps[bank][:, :], lhsT=gtT[:],
                             rhs=bdf[:, qd * 512:(qd + 1) * 512], start=True, stop=True)
                        ya = yacc[:, ts, qd * 512:(qd + 1) * 512]
                        S.op("dve", [bn, "yacc"], ["yacc"], nc.vector.tensor_tensor, out=ya, in0=ps[bank][:, :],
                             in1=ya, op=ALU.add)
                    S.op("dve", ["yacc", "g2b"], ["yo"], nc.vector.tensor_tensor, out=yo[:], in0=yacc[:, ts, :],
                         in1=g2b[:, row, :], op=ALU.mult)
                    S.op("dve", ["yo", "x1r"], ["yo"], nc.vector.tensor_tensor, out=yo[:], in0=yo[:], in1=x1r[:],
                         op=ALU.add)
                    S.op("dve", [], ["sse"], nc.vector.memset, ssq[:], 0.0)
                    S.op("act", ["yo", "sse"], ["x1r", "sse"], nc.scalar.activation,
                         out=x1r[:].bitcast(BF16)[:, 0:D], in_=yo[:], func=AF.Square, accum_out=ssq[:, 0:1])
                    S.op("act", ["sse"], ["rse"], nc.scalar.activation, out=rst[:], in_=ssq[:], func=AF.Sqrt,
                         bias=epsc[:, 0:1], scale=1.0 / D)
                    S.op("dve", ["rse"], ["rse"], nc.vector.reciprocal, out=rst[:], in_=rst[:])
                    S.op("dve", ["yo", "rse", "gfb"], ["yo"], nc.vector.scalar_tensor_tensor, out=yo[:], in0=yo[:],
                         scalar=rst[:, 0:1], in1=gfb[:], op0=ALU.mult, op1=ALU.mult)
                    dst = y_smp if row else y_own[r0:r0 + 128, :]
                    S.dma("sp", "yod", ["yo"], [], dst, yo[:])
        S.mute = False
        stats = S.emit()
        nc._mk_stats = stats
    return nc


def _nt(S, nc, names, t, src_ap, row, hT, col0, hn, g1p, sh1, epsc, identb, psb):
    xt, xb, ssq, rst, junk = t["xt"], t["xb"], t["ssq"], t["rst"], t["junk"]
    _, xtn, xbn, ssn, rsn, jn = names
    S.dma("sp", xtn, [], [xtn], xt[:], src_ap)
    S.op("dve", [], [ssn], nc.vector.memset, ssq[:], 0.0)
    S.op("act", [xtn, ssn], [jn, ssn], nc.scalar.activation, out=junk[:], in_=xt[:], func=AF.Square,
         accum_out=ssq[:, 0:1])
    S.op("act", [ssn], [rsn], nc.scalar.activation, out=rst[:], in_=ssq[:], func=AF.Sqrt,
         bias=epsc[:, 0:1], scale=1.0 / D)
    S.op("dve", [rsn], [rsn], nc.vector.reciprocal, out=rst[:], in_=rst[:])
    S.op("dve", [xtn, rsn], [xbn], nc.vector.tensor_scalar, out=xb[:], in0=xt[:], scalar1=rst[:, 0:1],
         scalar2=None, op0=ALU.mult)
    for half in range(2):
        bank = 6 + half
        bn = "ps%d" % bank
        for k in range(8):
            kc = half * 8 + k
            S.op("pe", [xbn, "identb"], [bn], nc.tensor.transpose, psb(bank)[:, k * 128:(k + 1) * 128],
                 xb[:, kc * 128:(kc + 1) * 128], identb[:])
        dst = hT[:, half * 8:(half + 1) * 8, col0:col0 + 128]
        S.op("dve", [bn, "g1p"], [hn], nc.vector.tensor_tensor, out=dst,
             in0=psb(bank).rearrange("p (k t) -> p k t", k=8),
             in1=g1p[:, row, half * 8:(half + 1) * 8].unsqueeze(2).broadcast_to([128, 8, 128]),
             op=ALU.mult)
        S.op("dve", [hn, "sh1"], [hn], nc.vector.tensor_tensor, out=dst, in0=dst,
             in1=sh1[:, row, half * 8:(half + 1) * 8].unsqueeze(2).broadcast_to([128, 8, 128]),
             op=ALU.add)


def _tables(cfg, j):
    NBS, NSLOT, TQ, SK, PAST = cfg.NBS, cfg.NSLOT, cfg.TQ, cfg.SK, cfg.PAST
    SEQ = NBS * 128
    slopes = 2.0 ** (-(np.arange(1, NH + 1)))
    pos = np.arange(SEQ)
    kaug = np.zeros((NH, 4, SEQ), np.float32)
    kaug_s = np.zeros((NH, 4, SK), np.float32)
    qaug = np.zeros((NH, 4, TQ), np.float32)
    poss = np.arange(SK)
    qpos = np.concatenate([(4 * m + j) * 128 + np.arange(128) for m in range(NSLOT)] + [PAST + np.arange(128)])
    for h in range(NH):
        s = slopes[h]
        kaug[h, 0] = s * 128.0 * (pos // 128); kaug[h, 1] = s * (pos % 128); kaug[h, 2] = 1.0; kaug[h, 3] = 1.0
        kaug_s[h, 0] = s * 128.0 * (poss // 128); kaug_s[h, 1] = s * (poss % 128); kaug_s[h, 2] = 1.0; kaug_s[h, 3] = 1.0
        qaug[h, 0] = 1.0; qaug[h, 1] = 1.0
        qaug[h, 2] = -s * 128.0 * (qpos // 128); qaug[h, 3] = -s * (qpos % 128)
    kk = np.arange(128)[:, None]
    qq = np.arange(128)[None, :]
    gb = np.zeros((128, NH, 4, 128), np.float32)
    gbs = np.zeros((128, NH, 128), np.float32)
    for h in range(NH):
        s = slopes[h]
        for i in range(4):
            if i < j:
                m_ = np.zeros((128, 128), np.float32)
            elif i > j:
                m_ = np.full((128, 128), NEG, np.float32)
            else:
                d = (qq - kk).astype(np.float32)
                m_ = 2.0 * s * np.minimum(d, 0.0)
                m_ = np.where((kk // 64) <= (qq // 64), m_, NEG).astype(np.float32)
            gb[:, h, i, :] = m_
        d = (qq - kk).astype(np.float32)
        m_ = 2.0 * s * np.minimum(d, 0.0)
        m_ = np.where(kk < 64, m_, NEG).astype(np.float32)
        gbs[:, h, :] = m_
    hm = np.ones((1, 128), np.float32)
    if j == 0:
        hm[0, 0:2] = 0.0
    return dict(kaug=kaug, kaug_s=kaug_s, qaug=qaug, gbias=gb.reshape(128, -1), gbias_s=gbs.reshape(128, -1),
                hmask=hm, ident=np.eye(128, dtype=np.float32))


_NC_CACHE = {}
_DEBUG = dict(stop=9, cores=8)


def kernel(x_prompt, x_sample, cache_k, cache_v, state_conv, c_prompt, c_sample,
           g_mix, g_ffn, w_ada, b_ada, w_in, lambda_q1, lambda_k1, lambda_q2, lambda_k2,
           subln_g, conv_w, w_o, w_router, b_router, w_gu, b_gu, w_down, b_down, g_final):
    f = lambda a: np.ascontiguousarray(np.asarray(a, dtype=np.float32))
    x_prompt, x_sample = f(x_prompt), f(x_sample)
    B, SEQ, _ = x_prompt.shape
    NE = int(np.asarray(w_router).shape[-1])
    PAST = int(np.asarray(cache_k).shape[2])
    cfg = Cfg(nbs=SEQ // 128, ne=NE, past=PAST, stop=_DEBUG["stop"])
    key = (cfg.NBS, cfg.NE, cfg.PAST, cfg.stop)
    if key not in _NC_CACHE:
        _NC_CACHE[key] = build(cfg)
    nc = _NC_CACHE[key]
    NSLOT = cfg.NSLOT
    common = dict(
        g_mix=f(g_mix)[0], g_ffn=f(g_ffn)[0], g_final=f(g_final).reshape(1, D),
        w_ada=f(w_ada)[0], b_ada=f(b_ada)[0], w_in=f(w_in)[0],
        lam4=np.stack([f(lambda_q1)[0], f(lambda_k1)[0], f(lambda_q2)[0], f(lambda_k2)[0]]),
        subln_g=f(subln_g)[0].reshape(1, 128), conv_w=f(conv_w)[0], w_o=f(w_o)[0],
        w_router=f(w_router)[0], b_router=f(b_router)[0].reshape(1, NE),
        w_gu=f(w_gu)[0], b_gu=f(b_gu)[0], w_down=f(w_down)[0], b_down=f(b_down)[0])
    ck, cv, sc = f(cache_k)[0], f(cache_v)[0], f(state_conv)[0]
    cp, cs = f(c_prompt), f(c_sample)
    in_maps = []
    own_rows = []
    for c in range(8):
        b, j = c // 4, c % 4
        rows = np.concatenate([(4 * m + j) * 128 + np.arange(128) for m in range(NSLOT)])
        own_rows.append(rows)
        hrows = []
        for m in range(NSLOT):
            p0 = (4 * m + j) * 128
            hrows += [max(p0 - 2, 0), max(p0 - 1, 0)]
        hrows = (hrows + [0] * 128)[:128]
        m_ = dict(common)
        m_.update(_tables(cfg, j))
        m_.update(
            x_all=x_prompt[b], x_own=np.ascontiguousarray(x_prompt[b][rows]),
            x_halo=np.ascontiguousarray(x_prompt[b][np.asarray(hrows)]),
            x_smp=np.concatenate([x_sample[c], np.zeros((64, D), np.float32)], axis=0),
            c2=np.stack([cp[b], cs[c]]),
            cache_k=ck[c].reshape(PAST, AW), cache_v=cv[c].reshape(PAST, AW), state_conv=sc[c])
        in_maps.append(m_)
    ncores = _DEBUG["cores"]
    res = run_bass_kernel_spmd(nc, in_maps[:ncores], core_ids=list(range(ncores)))
    R = list(res.results) + [res.results[0]] * (8 - ncores)
    y_prompt = np.zeros((B, SEQ, D), np.float32)
    k_prompt = np.zeros((1, B, SEQ, NH, 2, 64), np.float32)
    v_prompt = np.zeros((1, B, SEQ, NH, 128), np.float32)
    conv_prompt = np.zeros((1, B, 2, CW), np.float32)
    y_sample = np.zeros((8, 64, D), np.float32)
    k_sample = np.zeros((1, 8, 64, NH, 2, 64), np.float32)
    v_sample = np.zeros((1, 8, 64, NH, 128), np.float32)
    conv_sample = np.zeros((1, 8, 2, CW), np.float32)
    for c in range(8):
        b, j = c // 4, c % 4
        r = R[c]
        rows = own_rows[c]
        y_prompt[b, rows] = r["y_own"]
        k_prompt[0, b, rows] = r["k_own"].reshape(-1, NH, 2, 64)
        v_prompt[0, b, rows] = r["v_own"].reshape(-1, NH, 128)
        if j == 3:
            conv_prompt[0, b] = r["conv_p"]
        y_sample[c] = r["y_smp"][:64]
        k_sample[0, c] = r["k_smp"][:64].reshape(64, NH, 2, 64)
        v_sample[0, c] = r["v_smp"][:64].reshape(64, NH, 128)
        conv_sample[0, c] = r["conv_s"]
    return (y_prompt, y_sample, k_prompt, v_prompt, conv_prompt, k_sample, v_sample, conv_sample)
```
